# Optimizing a Trainium2 kernel written in Bass

```python
import jax
import jax.numpy as jnp
from jax import lax
import numpy as np

D_MODEL = 1024
BATCH = 16
SEQ = 2048
DEPTH = 2

CTX_LEN = 256
GRID_W = 64
EPS = 1e-6
F32 = jnp.float32

MLSTM_HEADS = 4
MLSTM_HEAD_DIM = 64
MLSTM_WIDTH = MLSTM_HEADS * MLSTM_HEAD_DIM
MLSTM_CHUNK = 128
MLA_HEADS = 8
MLA_Q_RANK = 256
MLA_KV_RANK = 128
MLA_NOPE_DIM = 64
MLA_ROPE_DIM = 32
MLA_V_DIM = 64
MLA_QK_DIM = MLA_NOPE_DIM + MLA_ROPE_DIM
MLA_WIDTH = MLA_HEADS * MLA_V_DIM
ROPE_BASE = 10000.0
ATTN_BLOCK = 128
GMLP_GROUPS = 4
GMLP_GROUP_DIM = 64
GMLP_WIDTH = GMLP_GROUPS * GMLP_GROUP_DIM
GMLP_CHUNK = 128
MIX_WIDTH = MLSTM_WIDTH + MLA_WIDTH + GMLP_WIDTH

OFF_QA = 0
OFF_KA = OFF_QA + MLSTM_WIDTH
OFF_VA = OFF_KA + MLSTM_WIDTH
OFF_OA = OFF_VA + MLSTM_WIDTH
OFF_GA = OFF_OA + MLSTM_WIDTH
OFF_CQ = OFF_GA + 4 * MLSTM_HEADS
OFF_CKV = OFF_CQ + MLA_Q_RANK
OFF_KR = OFF_CKV + MLA_KV_RANK
OFF_GM = OFF_KR + MLA_ROPE_DIM
IN_COLS = OFF_GM + 2 * GMLP_WIDTH

D_FF = 2816
N_EXPERTS = 8
TOP_K = 2
D_FF_EXPERT = 3584
MOE_BLOCK = 512
N_DENSE = (DEPTH + 1) // 2
N_MOE = DEPTH // 2

kernel_name = 'hybrid_mlstm_mla_gmlp_moe_block'


def rms_norm(x, g):
    xf = x.astype(F32)
    y = xf * lax.rsqrt(jnp.mean(xf * xf, axis=-1, keepdims=True) + EPS)
    return y.astype(x.dtype) * g


def layer_norm(x, g, b):
    xf = x.astype(F32)
    mu = jnp.mean(xf, axis=-1, keepdims=True)
    var = jnp.mean(jnp.square(xf - mu), axis=-1, keepdims=True)
    return ((xf - mu) * lax.rsqrt(var + EPS)).astype(x.dtype) * g + b


def axial_rope(rows):
    row = jnp.repeat(jnp.arange(rows), GRID_W).astype(F32)
    col = jnp.tile(jnp.arange(GRID_W), rows).astype(F32)
    n_freq = MLA_ROPE_DIM // 4
    inv = ROPE_BASE ** (-jnp.arange(n_freq, dtype=F32) / n_freq)
    ang = jnp.concatenate([row[:, None] * inv, col[:, None] * inv], axis=-1)
    return jnp.cos(ang), jnp.sin(ang)


def apply_rope(t, cos, sin):
    half = t.shape[-1] // 2
    t1, t2 = t[..., :half], t[..., half:]
    cos = cos[None, :, None, :].astype(t.dtype)
    sin = sin[None, :, None, :].astype(t.dtype)
    return jnp.concatenate([t1 * cos - t2 * sin, t1 * sin + t2 * cos], axis=-1)


def mlstm_scan(q, k, v, log_i, log_f, state, with_h):
    bsz, t_len, heads, e = q.shape
    L = MLSTM_CHUNK
    nc = t_len // L

    def chunks(a):
        return jnp.moveaxis(a.reshape((bsz, nc, L) + a.shape[2:]), 1, 0)

    scan_order = jnp.tril(jnp.ones((L, L), bool))

    def step(carry, inp):
        C, n, m = carry
        qc, kc, vc, ic, fc = inp
        b = jnp.cumsum(fc, axis=1).transpose(0, 2, 1)
        ig = ic.transpose(0, 2, 1)
        b_last = b[:, :, -1]
        g = b_last[..., None] - b + ig
        m_new = jnp.maximum(b_last + m, jnp.max(g, axis=-1))
        w = jnp.exp(g - m_new[..., None])
        decay = jnp.exp(b_last + m - m_new)
        C_new = decay[..., None, None] * C + jnp.einsum('bhs,bshd,bshe->bhde', w, kc, vc)
        n_new = decay[..., None] * n + jnp.einsum('bhs,bshd->bhd', w, kc)
        if not with_h:
            return (C_new, n_new, m_new), None
        dmat = b[..., :, None] - b[..., None, :] + ig[..., None, :]
        dmat = jnp.where(scan_order, dmat, -jnp.inf)
        inter = b + m[..., None]
        m_t = jnp.maximum(inter, jnp.max(dmat, axis=-1))
        s = jnp.einsum('blhd,bshd->bhls', qc, kc) * jnp.exp(dmat - m_t[..., None])
        a = jnp.exp(inter - m_t)
        num = jnp.einsum('bhls,bshe->bhle', s, vc) + a[..., None] * jnp.einsum('blhd,bhde->bhle', qc, C)
        den = jnp.sum(s, axis=-1) + a * jnp.einsum('blhd,bhd->bhl', qc, n)
        h = num / jnp.maximum(jnp.abs(den), jnp.exp(-m_t))[..., None]
        return (C_new, n_new, m_new), h.transpose(0, 2, 1, 3)

    state, hs = lax.scan(step, state, (chunks(q), chunks(k), chunks(v), chunks(log_i), chunks(log_f)))
    if not with_h:
        return state, None
    return state, jnp.moveaxis(hs, 0, 1).reshape(bsz, t_len, heads, e)


def mlstm_split(z, gate_b):
    bsz, t_len, _ = z.shape

    def heads(a):
        return a.reshape(bsz, t_len, MLSTM_HEADS, MLSTM_HEAD_DIM).astype(F32)

    q = heads(z[..., OFF_QA:OFF_KA])
    k = heads(z[..., OFF_KA:OFF_VA]) * (MLSTM_HEAD_DIM ** -0.5)
    v = heads(z[..., OFF_VA:OFF_OA])
    o = jax.nn.sigmoid(z[..., OFF_OA:OFF_GA])
    gates = (z[..., OFF_GA:OFF_CQ] + gate_b).astype(F32).reshape(bsz, t_len, 4, MLSTM_HEADS)
    log_i = gates[:, :, 0:2]
    log_f = jax.nn.log_sigmoid(gates[:, :, 2:4])
    return q, k, v, o, log_i, log_f


def mlstm_mixer(z_lat, z_ctx, gate_b, norm_g, with_ctx_out):
    ql, kl, vl, ol, il, fl = mlstm_split(z_lat, gate_b)
    qc, kc, vc, oc, ic, fc = mlstm_split(z_ctx, gate_b)
    bsz = z_lat.shape[0]
    zero_state = (jnp.zeros((bsz, MLSTM_HEADS, MLSTM_HEAD_DIM, MLSTM_HEAD_DIM), F32),
                  jnp.zeros((bsz, MLSTM_HEADS, MLSTM_HEAD_DIM), F32),
                  jnp.zeros((bsz, MLSTM_HEADS), F32))
    g = norm_g.reshape(MLSTM_HEADS, MLSTM_HEAD_DIM)
    h_lat = 0.0
    h_ctx = 0.0
    for d in range(2):
        rev = d == 1
        fl_ = (lambda a: jnp.flip(a, axis=1)) if rev else (lambda a: a)
        ctx_state, hc = mlstm_scan(fl_(qc), fl_(kc), fl_(vc), fl_(ic[:, :, d]), fl_(fc[:, :, d]),
                                   zero_state, with_ctx_out)
        _, hl = mlstm_scan(fl_(ql), fl_(kl), fl_(vl), fl_(il[:, :, d]), fl_(fl[:, :, d]), ctx_state, True)
        h_lat = h_lat + fl_(hl)
        if with_ctx_out:
            h_ctx = h_ctx + fl_(hc)
    bl, tl = z_lat.shape[:2]
    out_lat = (ol * rms_norm(h_lat, g).reshape(bl, tl, MLSTM_WIDTH)).astype(z_lat.dtype)
    if not with_ctx_out:
        return out_lat, None
    tc = z_ctx.shape[1]
    out_ctx = (oc * rms_norm(h_ctx, g).reshape(bl, tc, MLSTM_WIDTH)).astype(z_ctx.dtype)
    return out_lat, out_ctx


def with_rope(t, rope):
    return jnp.concatenate([t[..., :MLA_NOPE_DIM], apply_rope(t[..., MLA_NOPE_DIM:], rope[0], rope[1])], axis=-1)


def mla_queries(z, cq_g, w_uq, q_g, rope):
    bsz, t_len, _ = z.shape
    q = (rms_norm(z[..., OFF_CQ:OFF_CKV], cq_g) @ w_uq).reshape(bsz, t_len, MLA_HEADS, MLA_QK_DIM)
    q = rms_norm(q, q_g)
    return q if rope is None else with_rope(q, rope)


def mla_keys_values(z, ckv_g, w_ukv, k_g, rope):
    bsz, t_len, _ = z.shape
    kv = (rms_norm(z[..., OFF_CKV:OFF_KR], ckv_g) @ w_ukv).reshape(bsz, t_len, MLA_HEADS, MLA_NOPE_DIM + MLA_V_DIM)
    k_rope = jnp.broadcast_to(z[..., None, OFF_KR:OFF_GM], (bsz, t_len, MLA_HEADS, MLA_ROPE_DIM))
    k = rms_norm(jnp.concatenate([kv[..., :MLA_NOPE_DIM], k_rope], axis=-1), k_g)
    v = kv[..., MLA_NOPE_DIM:]
    k = k if rope is None else with_rope(k, rope)
    return k, v


def attention(q, k, v):
    s = jnp.einsum('bqhd,bkhd->bhqk', q, k).astype(F32) * (MLA_QK_DIM ** -0.5)
    p = jax.nn.softmax(s, axis=-1).astype(v.dtype)
    return jnp.einsum('bhqk,bkhd->bqhd', p, v)


def blocked_attention(q, k, v):
    bsz, t_len, heads, dq = q.shape
    nb = t_len // ATTN_BLOCK
    qb = jnp.moveaxis(q.reshape(bsz, nb, ATTN_BLOCK, heads, dq), 1, 0)
    ob = lax.map(lambda qq: attention(qq, k, v), qb)
    return jnp.moveaxis(ob, 0, 1).reshape(bsz, t_len, heads * v.shape[-1])


def gmlp_mixer(z, ln_g, ln_b, w_s, b_s):
    bsz, t_len, _ = z.shape
    a = jax.nn.gelu(z[..., OFF_GM:IN_COLS])
    u, v = a[..., :GMLP_WIDTH], a[..., GMLP_WIDTH:]
    v = layer_norm(v, ln_g, ln_b).reshape(bsz, t_len // GMLP_CHUNK, GMLP_CHUNK, GMLP_GROUPS, GMLP_GROUP_DIM)
    sv = jnp.einsum('gpq,bcqgd->bcpgd', w_s, v) + b_s.T[None, None, :, :, None]
    return u * sv.reshape(bsz, t_len, GMLP_WIDTH)


def swiglu(h, w1, w3, w2):
    return (jax.nn.silu(h @ w1) * (h @ w3)) @ w2


def moe_swiglu(h, router_w, router_b, w1, w3, w2):
    d = h.shape[-1]
    hf = h.reshape(-1, d)
    n = hf.shape[0]
    logits = (hf @ router_w).astype(F32) + router_b.astype(F32)
    top_logit, top_idx = lax.top_k(logits, TOP_K)
    gates = jax.nn.softmax(top_logit, axis=-1).astype(h.dtype)
    flat_e = top_idx.reshape(-1)
    flat_tok = jnp.repeat(jnp.arange(n, dtype=jnp.int32), TOP_K)
    order = jnp.argsort(flat_e)
    sorted_e = flat_e[order]
    counts = jnp.bincount(flat_e, length=N_EXPERTS)
    padded = (counts + MOE_BLOCK - 1) // MOE_BLOCK * MOE_BLOCK
    padded_end = jnp.cumsum(padded)
    rank = jnp.arange(n * TOP_K) - (jnp.cumsum(counts) - counts)[sorted_e]
    dest = (padded_end - padded)[sorted_e] + rank
    cap = (n * TOP_K + MOE_BLOCK - 1) // MOE_BLOCK * MOE_BLOCK + N_EXPERTS * MOE_BLOCK
    slot_tok = jnp.zeros((cap,), jnp.int32).at[dest].set(flat_tok[order])
    slot_gate = jnp.zeros((cap,), h.dtype).at[dest].set(gates.reshape(-1)[order])
    n_blocks = cap // MOE_BLOCK
    block_expert = jnp.minimum(
        jnp.searchsorted(padded_end, jnp.arange(n_blocks) * MOE_BLOCK, side='right'), N_EXPERTS - 1)
    xb = hf[slot_tok].reshape(n_blocks, MOE_BLOCK, d)

    def expert_block(args):
        xe, e = args
        return swiglu(xe, w1[e], w3[e], w2[e])

    yb = lax.map(expert_block, (xb, block_expert))
    y = jax.ops.segment_sum(yb.reshape(cap, d) * slot_gate[:, None], slot_tok, num_segments=n)
    return y.reshape(h.shape)


def channel_mixer(layer, h, ffn_w1, ffn_w3, ffn_w2, moe_router_w, moe_router_b, moe_w1, moe_w3, moe_w2):
    j = layer // 2
    if layer % 2 == 0:
        return swiglu(h, ffn_w1[j], ffn_w3[j], ffn_w2[j])
    return moe_swiglu(h, moe_router_w[j], moe_router_b[j], moe_w1[j], moe_w3[j], moe_w2[j])


def setup_inputs(seed: int = 0) -> dict:
    key = jax.random.key(seed)
    ks = iter(jax.random.split(key, 40))
    D = D_MODEL

    def nrm(shape, scale):
        return jax.random.normal(next(ks), shape, F32) * scale

    def gain(shape):
        return 1.0 + nrm(shape, 0.02)

    f_bias = jnp.tile(jnp.linspace(3.0, 6.0, MLSTM_HEADS), 2)[None, :] + nrm((DEPTH, 2 * MLSTM_HEADS), 0.1)
    i_bias = nrm((DEPTH, 2 * MLSTM_HEADS), 0.1)
    return {
        'x': nrm((BATCH, SEQ, D), 1.0),
        'c': nrm((BATCH, D), 1.0),
        'ctx': nrm((BATCH, CTX_LEN, D), 1.0),
        'c_ctx': nrm((D,), 1.0),
        'ada_w': nrm((DEPTH, D, 6 * D), 0.5 * D ** -0.5),
        'ada_b': nrm((DEPTH, 6 * D), 0.02),
        'norm1_g': gain((DEPTH, D)),
        'norm2_g': gain((DEPTH, D)),
        'w_in': nrm((DEPTH, D, IN_COLS), D ** -0.5),
        'w_out': nrm((DEPTH, MIX_WIDTH, D), MIX_WIDTH ** -0.5),
        'mlstm_gate_b': jnp.concatenate([i_bias, f_bias], axis=-1),
        'mlstm_norm_g': gain((DEPTH, MLSTM_WIDTH)),
        'mla_cq_g': gain((DEPTH, MLA_Q_RANK)),
        'mla_ckv_g': gain((DEPTH, MLA_KV_RANK)),
        'mla_w_uq': nrm((DEPTH, MLA_Q_RANK, MLA_HEADS * MLA_QK_DIM), MLA_Q_RANK ** -0.5),
        'mla_w_ukv': nrm((DEPTH, MLA_KV_RANK, MLA_HEADS * (MLA_NOPE_DIM + MLA_V_DIM)), MLA_KV_RANK ** -0.5),
        'mla_q_g': gain((DEPTH, MLA_QK_DIM)),
        'mla_k_g': gain((DEPTH, MLA_QK_DIM)),
        'gmlp_ln_g': gain((DEPTH, GMLP_WIDTH)),
        'gmlp_ln_b': nrm((DEPTH, GMLP_WIDTH), 0.02),
        'gmlp_w_s': nrm((DEPTH, GMLP_GROUPS, GMLP_CHUNK, GMLP_CHUNK), GMLP_CHUNK ** -0.5),
        'gmlp_b_s': gain((DEPTH, GMLP_GROUPS, GMLP_CHUNK)),
        'ffn_w1': nrm((N_DENSE, D, D_FF), D ** -0.5),
        'ffn_w3': nrm((N_DENSE, D, D_FF), D ** -0.5),
        'ffn_w2': nrm((N_DENSE, D_FF, D), D_FF ** -0.5),
        'moe_router_w': nrm((N_MOE, D, N_EXPERTS), D ** -0.5),
        'moe_router_b': nrm((N_MOE, N_EXPERTS), 0.01),
        'moe_w1': nrm((N_MOE, N_EXPERTS, D, D_FF_EXPERT), D ** -0.5),
        'moe_w3': nrm((N_MOE, N_EXPERTS, D, D_FF_EXPERT), D ** -0.5),
        'moe_w2': nrm((N_MOE, N_EXPERTS, D_FF_EXPERT, D), D_FF_EXPERT ** -0.5),
    }


def reference(x, c, ctx, c_ctx, ada_w, ada_b, norm1_g, norm2_g, w_in, w_out, mlstm_gate_b, mlstm_norm_g,
              mla_cq_g, mla_ckv_g, mla_w_uq, mla_w_ukv, mla_q_g, mla_k_g, gmlp_ln_g, gmlp_ln_b, gmlp_w_s,
              gmlp_b_s, ffn_w1, ffn_w3, ffn_w2, moe_router_w, moe_router_b, moe_w1, moe_w3, moe_w2):
    rows = x.shape[1] // GRID_W
    rope = axial_rope(rows)
    ffn = (ffn_w1, ffn_w3, ffn_w2, moe_router_w, moe_router_b, moe_w1, moe_w3, moe_w2)
    xc = ctx
    for l in range(DEPTH):
        last = l == DEPTH - 1
        sh1, sc1, g1, sh2, sc2, g2 = jnp.split((jax.nn.silu(c) @ ada_w[l] + ada_b[l])[:, None, :], 6, axis=-1)
        sh1c, sc1c, g1c, sh2c, sc2c, g2c = jnp.split(jax.nn.silu(c_ctx) @ ada_w[l] + ada_b[l], 6, axis=-1)
        z = (rms_norm(x, norm1_g[l]) * (1.0 + sc1) + sh1) @ w_in[l]
        zc = (rms_norm(xc, norm1_g[l]) * (1.0 + sc1c) + sh1c) @ w_in[l]
        a_lat, a_ctx = mlstm_mixer(z, zc, mlstm_gate_b[l], mlstm_norm_g[l], not last)
        k_lat, v_lat = mla_keys_values(z, mla_ckv_g[l], mla_w_ukv[l], mla_k_g[l], rope)
        k_ctx, v_ctx = mla_keys_values(zc, mla_ckv_g[l], mla_w_ukv[l], mla_k_g[l], None)
        q_lat = mla_queries(z, mla_cq_g[l], mla_w_uq[l], mla_q_g[l], rope)
        b_lat = blocked_attention(q_lat, jnp.concatenate([k_lat, k_ctx], axis=1),
                                  jnp.concatenate([v_lat, v_ctx], axis=1))
        c_lat = gmlp_mixer(z, gmlp_ln_g[l], gmlp_ln_b[l], gmlp_w_s[l], gmlp_b_s[l])
        x = x + g1 * (jnp.concatenate([a_lat, b_lat, c_lat], axis=-1) @ w_out[l])
        x = x + g2 * channel_mixer(l, rms_norm(x, norm2_g[l]) * (1.0 + sc2) + sh2, *ffn)
        if not last:
            q_ctx = mla_queries(zc, mla_cq_g[l], mla_w_uq[l], mla_q_g[l], None)
            b_ctx = attention(q_ctx, k_ctx, v_ctx).reshape(xc.shape[0], xc.shape[1], MLA_WIDTH)
            c_ctx_out = gmlp_mixer(zc, gmlp_ln_g[l], gmlp_ln_b[l], gmlp_w_s[l], gmlp_b_s[l])
            xc = xc + g1c * (jnp.concatenate([a_ctx, b_ctx, c_ctx_out], axis=-1) @ w_out[l])
            xc = xc + g2c * channel_mixer(l, rms_norm(xc, norm2_g[l]) * (1.0 + sc2c) + sh2c, *ffn)
    return x
```

```python
from collections import defaultdict
from contextlib import ExitStack
import concourse.bass as bass
import concourse.mybir as mybir

F32 = mybir.dt.float32
BF16 = mybir.dt.bfloat16
U8 = mybir.dt.uint8
I32 = mybir.dt.int32
AF = mybir.ActivationFunctionType
ALU = mybir.AluOpType
AX = mybir.AxisListType
ESZ = {F32: 4, BF16: 2, U8: 1, I32: 4}

ENGS = ["pe", "act", "dve", "pool", "sp"]
WRITE_KW = ("out", "accum_out", "ap")
GRAN = 512
SEM_MAX = 30000
DO_CLEAR = False


def is_ap(v):
    return hasattr(v, "ap") and hasattr(v, "offset") and hasattr(v, "tensor")


def ap_box(ap):
    t = ap.tensor
    tn = type(t).__name__
    if tn.startswith("SB"):
        space = "sb:" + t.name
    elif tn.startswith("PSum"):
        space = "ps:" + t.name
    else:
        return None
    pairs = [tuple(p) for p in ap.ap]
    es = ESZ[ap.dtype]
    rowlen = int(t.shape[1]) if len(t.shape) == 2 else None
    pstep, pcnt = pairs[0]
    if pstep == 0:
        pstep = rowlen
    assert pstep == rowlen, (pstep, rowlen, ap)
    off = int(ap.offset)
    p0 = off // rowlen
    c0 = off % rowlen
    ext = 0
    for st, cn in pairs[1:]:
        ext += abs(int(st)) * (int(cn) - 1)
    b0 = c0 * es
    b1 = (c0 + ext + 1) * es
    return (space, p0, p0 + int(pcnt), b0, b1)


class Op:
    __slots__ = ("eng", "fn", "waits", "tok", "dsem", "kind", "arg")

    def __init__(self, eng, fn, kind="ins"):
        self.eng = eng
        self.fn = fn
        self.waits = []
        self.tok = None
        self.dsem = None
        self.kind = kind
        self.arg = None


class Prog:
    def __init__(self, nc, n_esem=40, n_dsem=48, same_eng_sync=True):
        self.nc = nc
        self.ops = {e: [] for e in ENGS}
        self.seen = {e: {} for e in ENGS}
        self.same = same_eng_sync
        self.buckets = defaultdict(lambda: defaultdict(list))
        self.rdedupe = {}
        self.psum = defaultdict(dict)
        self.keys = {}
        self.n_esem = n_esem
        self.n_dsem = n_dsem
        self.dcount = [0] * n_dsem
        self.dnext = 0
        self.epoch = 0
        self.ep_bounds = {e: [0] for e in ENGS}
        self.n_ops = 0

    def _need(self, E, tok, deps):
        if tok is None:
            return
        if tok[0] == "e":
            if tok[2] < self.ep_bounds[tok[1]][-1]:
                return
            if tok[1] == E and (E == "pe" or E == "sp" or not self.same):
                return
            k = ("e", tok[1])
            v = tok[2]
        else:
            if tok[1] < self.epoch:
                return
            k = ("d", tok[2])
            v = tok[3]
        if self.seen[E].get(k, -1) >= v:
            return
        if deps.get(k, -1) < v:
            deps[k] = v

    def _track_sb(self, E, box, is_w, deps, newrecs):
        space, p0, p1, b0, b1 = box
        bk = self.buckets[space]
        for bi in range(b0 // GRAN, (b1 - 1) // GRAN + 1):
            lst = bk[bi]
            keep = []
            for rec in lst:
                if not rec[6]:
                    continue
                if rec[0] < p1 and p0 < rec[1] and rec[2] < b1 and b0 < rec[3]:
                    if is_w or rec[4]:
                        self._need(E, rec[5], deps)
                    if is_w and p0 <= rec[0] and rec[1] <= p1 and b0 <= rec[2] and rec[3] <= b1:
                        rec[6] = False
                        continue
                keep.append(rec)
            bk[bi] = keep
        newrecs.append((box, is_w))

    def _commit_sb(self, E, tok, newrecs):
        for box, is_w in newrecs:
            space, p0, p1, b0, b1 = box
            if not is_w and tok[0] == "e":
                key = (box, E)
                old = self.rdedupe.get(key)
                if old is not None and old[6]:
                    old[5] = tok
                    continue
            rec = [p0, p1, b0, b1, is_w, tok, True]
            if not is_w and tok[0] == "e":
                self.rdedupe[(box, E)] = rec
            bk = self.buckets[space]
            for bi in range(b0 // GRAN, (b1 - 1) // GRAN + 1):
                bk[bi].append(rec)

    def _deps_for(self, E, R, W, RK, WK):
        deps = {}
        newrecs = []
        psb = []
        for ap, is_w in [(a, False) for a in R] + [(a, True) for a in W]:
            box = ap_box(ap)
            if box is None:
                continue
            if box[0].startswith("ps:"):
                for bank in range(box[3] // 2048, (box[4] - 1) // 2048 + 1):
                    key = (box[0], bank)
                    for F, tok in self.psum[key].items():
                        if F != E:
                            self._need(E, tok, deps)
                    psb.append(key)
            else:
                self._track_sb(E, box, is_w, deps, newrecs)
        for k in RK:
            st = self.keys.setdefault(k, {"w": None, "r": {}})
            self._need(E, st["w"], deps)
        for k in WK:
            st = self.keys.setdefault(k, {"w": None, "r": {}})
            self._need(E, st["w"], deps)
            for t in st["r"].values():
                self._need(E, t, deps)
        return deps, newrecs, psb

    def _finish(self, E, op, tok, deps, newrecs, psb, RK, WK):
        for k, v in deps.items():
            self.seen[E][k] = v
            op.waits.append((k, v, self.epoch))
        self._commit_sb(E, tok, newrecs)
        for key in psb:
            self.psum[key] = {E: tok}
        for k in RK:
            rk = E if tok[0] == "e" else ("d", tok[2])
            self.keys[k]["r"][rk] = tok
        for k in WK:
            self.keys[k]["w"] = tok
            self.keys[k]["r"] = {}
        self.ops[E].append(op)
        self.n_ops += 1

    def I(self, E, meth, RK=(), WK=(), **kw):
        R = [v for k, v in kw.items() if is_ap(v) and k not in WRITE_KW]
        W = [v for k, v in kw.items() if is_ap(v) and k in WRITE_KW]
        deps, newrecs, psb = self._deps_for(E, R, W, RK, WK)
        op = Op(E, lambda e: getattr(e, meth)(**kw))
        tok = ("e", E, len(self.ops[E]))
        op.tok = tok
        self._finish(E, op, tok, deps, newrecs, psb, RK, WK)
        return op

    def dma(self, Q, out, in_, RK=(), WK=(), **kw):
        i = self.dnext
        self.dnext = (self.dnext + 1) % self.n_dsem
        deps, newrecs, psb = self._deps_for(Q, [in_], [out], RK, WK)
        if self.dcount[i] > 0:
            self._need(Q, ("d", self.epoch, i, 16 * self.dcount[i]), deps)
        self.dcount[i] += 1
        tok = ("d", self.epoch, i, 16 * self.dcount[i])
        op = Op(Q, lambda e: e.dma_start(out=out, in_=in_, **kw))
        op.dsem = (self.epoch, i)
        op.tok = tok
        self._finish(Q, op, tok, deps, newrecs, psb, RK, WK)
        return op

    def barrier(self):
        toks = []
        for e in ENGS:
            lo = self.ep_bounds[e][-1]
            for idx in range(len(self.ops[e]) - 1, lo - 1, -1):
                t = self.ops[e][idx].tok
                if t is not None and t[0] == "e":
                    toks.append(t)
                    break
        for i in range(self.n_dsem):
            if self.dcount[i]:
                toks.append(("d", self.epoch, i, 16 * self.dcount[i]))
        bops = []
        for e in ENGS:
            deps = {}
            for t in toks:
                if t[0] == "e" and t[1] == e:
                    continue
                self._need(e, t, deps)
            op = Op(e, None, kind="barrier")
            op.waits = [(k, v, self.epoch) for k, v in deps.items()]
            bops.append(op)
        self.epoch += 1
        self.seen = {e: {} for e in ENGS}
        for e, op in zip(ENGS, bops):
            self.ep_bounds[e].append(len(self.ops[e]))
            op.tok = ("e", e, len(self.ops[e]))
            self.ops[e].append(op)
        if False:
            op = Op("sp", None, kind="clear")
            op.arg = (self.epoch + 1) % 3
            op.tok = ("e", "sp", len(self.ops["sp"]))
            self.ops["sp"].append(op)

    def emit(self):
        import bisect
        nc = self.nc
        self.barrier()
        nep = self.epoch + 1
        need = {e: set() for e in ENGS}
        for e in ENGS:
            for op in self.ops[e]:
                for (k, v, ep) in op.waits:
                    if k[0] == "e":
                        need[k[1]].add(v)
            for op in self.ops[e]:
                if op.kind in ("barrier", "clear"):
                    need[e].add(op.tok[2])
        ep_of = lambda e, idx: bisect.bisect_right(self.ep_bounds[e], idx) - 1
        val = {e: {} for e in ENGS}
        used = [0] * nep
        per_ep = defaultdict(lambda: defaultdict(list))
        for e in ENGS:
            for idx in sorted(need[e]):
                per_ep[ep_of(e, idx)][e].append(idx)
        maxuse = 0
        base = 0
        for e in ENGS:
            lst = sorted(need[e])
            for r, idx in enumerate(lst):
                val[e][idx] = (0, base + r // SEM_MAX, r % SEM_MAX + 1)
            base += (len(lst) + SEM_MAX - 1) // SEM_MAX
        assert base <= self.n_esem, f"need {base} engine semaphores > {self.n_esem}"
        maxuse = base
        self.stats = {e: (len(self.ops[e]), len(need[e])) for e in ENGS}
        self.stats["epochs"] = nep
        self.stats["max_esem"] = maxuse
        with ExitStack() as es:
            esem = [[es.enter_context(nc.semaphore(f"e{s}_{j}")) for j in range(self.n_esem)] for s in range(1)]
            dsem = [[es.enter_context(nc.semaphore(f"d{s}_{j}")) for j in range(self.n_dsem)] for s in range(1)]
            block = es.enter_context(nc.Block())

            def run(E):
                def body(eng):
                    for idx, op in enumerate(self.ops[E]):
                        for (k, v, ep) in op.waits:
                            if k[0] == "e":
                                st, si, sv = val[k[1]][v]
                                eng.wait_ge(esem[st][si], sv)
                            else:
                                eng.wait_ge(dsem[0][k[1]], v)
                        if op.kind == "ins":
                            ins = op.fn(eng)
                        elif op.kind == "barrier":
                            ins = eng.nop()
                        else:
                            if DO_CLEAR:
                                for j in range(self.n_esem):
                                    eng.sem_clear(esem[op.arg][j])
                                for j in range(self.n_dsem):
                                    eng.sem_clear(dsem[op.arg][j])
                            ins = eng.nop()
                        if op.dsem is not None:
                            ins.then_inc(dsem[0][op.dsem[1]], 16)
                        elif idx in val[E]:
                            st, si, sv = val[E][idx]
                            ins.then_inc(esem[st][si], 1)
                return body

            block.tensor(run("pe"))
            block.scalar(run("act"))
            block.vector(run("dve"))
            block.gpsimd(run("pool"))
            block.sync(run("sp"))


class Arena:
    def __init__(self, big, size):
        self.big = big
        self.size = size
        self.top = 0
        self.stack = []
        self.peak = 0

    def push(self):
        self.stack.append(self.top)

    def pop(self):
        self.top = self.stack.pop()

    def alloc(self, free_shape, dtype, parts=128):
        n = 1
        for s in free_shape:
            n *= s
        nb = n * ESZ[dtype]
        nb_al = (nb + 63) // 64 * 64
        off = self.top
        assert off + nb_al <= self.size, f"SBUF arena overflow: need {off + nb_al} > {self.size}"
        self.top += nb_al
        self.peak = max(self.peak, self.top)
        v = self.big[0:parts, off:off + nb].bitcast(dtype)
        if len(free_shape) == 1:
            return v
        names = " ".join(f"d{i}" for i in range(len(free_shape)))
        kw = {f"d{i}": free_shape[i] for i in range(1, len(free_shape))}
        return v.rearrange(f"p ({names}) -> p {names}", **kw)


import numpy as np
import ml_dtypes
from contextlib import ExitStack
import concourse.bass as bass
import concourse.mybir as mybir
from concourse.bass_utils import run_bass_kernel_spmd

D = 1024
S = 2048
CT = 256
NT = 18
NTOK = NT * 128
EPS = 1e-6
INC = 1968
DFF = 2816
DFE = 3584
NEG = -30000.0
ARENA = 207 * 1024
SAME_SYNC = True
N_DUMMY_C = 0
XQ = "sp"

NG, CQG, CKVG, QG, KG, LNG, LNB, GB, RB = 0, 256, 512, 640, 1408, 2176, 2432, 2688, 2704
NBC = 2712
NCOLS = 68


def v3(ap, a):
    return ap.rearrange("p (a b) -> p a b", a=a)


def build(nlayers=2, stop=None, dbg=False, with_moe=True):
    nc = bass.Bass("TRN2", target_bir_lowering=False)

    def din(name, shape, dt=F32):
        return nc.dram_tensor(name, shape, dt, kind="ExternalInput").ap()

    def dscr(name, shape, dt=F32):
        return nc.dram_tensor(name, shape, dt).ap()

    xin = din("xin", [2, S, D])
    cin = din("cin", [2, CT, D])
    cT_d = din("cT", [128, 24])
    consts_d = din("consts", [128, 770])
    sel_d = din("sel", [4, 1024])
    rope_d = din("rope", [128, 16 * 32])
    ada_w = din("ada_w", [2, D, 6 * D])
    w_in = din("w_in", [2, D, INC])
    w_out = din("w_out", [2, D, D])
    w_uq = din("w_uq", [2, 256, 768])
    w_ukv = din("w_ukv", [2, 128, 1024])
    gws = din("gws", [2, 128, 512])
    bc_d = din("bc", [2, 128, NBC])
    cols_d = din("cols", [2, 128, NCOLS])
    rows_d = din("rows", [2, 1, 6 * D])
    ffn_w1 = din("ffn_w1", [D, DFF])
    ffn_w3 = din("ffn_w3", [D, DFF])
    ffn_w2 = din("ffn_w2", [DFF, D])
    if with_moe:
        moe_w1 = din("moe_w1", [8, D, DFE])
        moe_w3 = din("moe_w3", [8, D, DFE])
        moe_w2 = din("moe_w2", [8, DFE, D])
        rw_d = din("rw", [128, 64])
    out_d = nc.dram_tensor("out", [2, S, D], F32, kind="ExternalOutput").ap()
    dbg_d = {}
    if dbg:
        for nm, shp in (("d_mixA", [128, 2 * NTOK]), ("d_mixB", [64, 8 * NTOK]), ("d_mixC", [128, 2 * NTOK]),
                        ("d_mod", [128, 144]), ("d_g1b", [128, 3 * 1024])):
            dbg_d[nm] = nc.dram_tensor(nm, shp, F32, kind="ExternalOutput").ap()
        dbg_d["d_xmid"] = nc.dram_tensor("d_xmid", [NTOK, D], F32, kind="ExternalOutput").ap()
        dbg_d["d_x1"] = nc.dram_tensor("d_x1", [2, S, D], F32, kind="ExternalOutput").ap()
        dbg_d["d_xc1"] = nc.dram_tensor("d_xc1", [2, CT, D], F32, kind="ExternalOutput").ap()

    x1_d = dbg_d["d_x1"] if dbg else dscr("x1", [2, S, D])
    xc1_d = dbg_d["d_xc1"] if dbg else dscr("xc1", [2, CT, D])
    xmid_d = dscr("xmid", [NT, 128, D])
    ml_qk_d = dscr("ml_qk", [NT, 128, 512], BF16)
    ml_kv_d = dscr("ml_kv", [NT, 128, 516], BF16)
    ml_og_d = dscr("ml_og", [NT, 128, 272])
    qt_d = dscr("qt", [96, 8, NTOK], BF16)
    kt_d = dscr("kt", [96, 8, NTOK], BF16)
    v1_d = dscr("v1", [NT, 128, 520], BF16)

    es = ExitStack()
    big = es.enter_context(nc.sbuf_tensor("big", [128, ARENA], U8))
    ps = es.enter_context(nc.psum_tensor("ps", [128, 4096], F32))
    psb = ps[:, :].bitcast(BF16)
    A = Arena(big, ARENA)
    P = Prog(nc, same_eng_sync=SAME_SYNC)

    def PSF(bank, a, b, parts=128, p0=0):
        return ps[p0:p0 + parts, bank * 512 + a: bank * 512 + b]

    def PSB(bank, a, b, parts=128):
        return psb[0:parts, bank * 1024 + a: bank * 1024 + b]

    dve = lambda m, **kw: P.I("dve", m, **kw)
    act = lambda m, **kw: P.I("act", m, **kw)
    pe = lambda m, **kw: P.I("pe", m, **kw)
    pool = lambda m, **kw: P.I("pool", m, **kw)

    def mm(out, lhsT, rhs, start=True, stop=True):
        pe("matmul", out=out, lhsT=lhsT, rhs=rhs, start=start, stop=stop)

    def rstd_from_ss(out, ss, n, tmp):
        act("activation", out=tmp, in_=ss, func=AF.Ln, scale=1.0 / n, bias=EPS)
        act("activation", out=out, in_=tmp, func=AF.Exp, scale=-0.5)

    consts = A.alloc([770], F32)
    P.dma("sp", consts, consts_d)
    identf = consts[:, 0:128]
    tri = [consts[:, 128:256], consts[:, 256:384]]
    mask = [consts[:, 384:512], consts[:, 512:640]]
    ones_f = consts[:, 640:768]
    hmask = consts[:, 768:770]
    identb = A.alloc([128], BF16)
    dve("tensor_copy", out=identb, in_=identf)
    selt = A.alloc([1024], F32, parts=4)
    P.dma("sp", selt, sel_d)
    sel4 = v3(selt[:, 0:512], 4)
    sel4n = v3(selt[:, 512:1024], 4)
    ropet = A.alloc([16, 32], F32)
    P.dma("sp", ropet, v3(rope_d, 16))
    sm = A.alloc([64], F32)
    junkb = A.alloc([1024], BF16)
    junkf = A.alloc([256], F32)

    for l in range(nlayers):
        last = l == 1
        A.push()
        bcL = A.alloc([NBC], F32)
        P.dma("sp", bcL, bc_d[l])
        colsL = A.alloc([NCOLS], F32)
        P.dma("sp", colsL, cols_d[l])
        modT = A.alloc([48, 3], F32)
        scl = A.alloc([2, 8, 3], F32)
        g1b = A.alloc([3, 1024], F32)
        g2b = A.alloc([3, 1024], F32)

        A.push()
        sT = A.alloc([24], F32)
        P.dma("sp", sT, cT_d)
        act("activation", out=sT, in_=sT, func=AF.Silu)
        brow = A.alloc([6 * D], F32, parts=1)
        P.dma("sp", brow, rows_d[l])
        modrow = A.alloc([6 * D], F32, parts=3)
        NWB = 4
        wbl = [A.alloc([8, 512], F32) for _ in range(NWB)]
        for jb in range(12):
            wb = wbl[jb % NWB]
            P.dma("sp" if jb % 2 == 0 else "act", wb,
                  ada_w[l][:, jb * 512:(jb + 1) * 512].rearrange("(kc p) n -> p kc n", p=128))
            bank = 1 + jb % 3
            o_ = PSF(bank, 0, 512, parts=3)
            for kc in range(8):
                mm(o_, sT[:, kc * 3:kc * 3 + 3], wb[:, kc, :], start=kc == 0, stop=False)
            mm(o_, ones_f[0:1, 0:3], brow[0:1, jb * 512:(jb + 1) * 512], start=False, stop=True)
            act("copy", out=modrow[:, jb * 512:(jb + 1) * 512], in_=o_)
            for jj in range(4):
                j = jb * 4 + jj
                pe("transpose", out=PSF(0, j * 3, j * 3 + 3), in_=modrow[0:3, j * 128:(j + 1) * 128], identity=identf[0:3, 0:3])
            if jb in (4, 5, 10, 11):
                gt = g1b if jb < 6 else g2b
                half = jb % 2
                for r in range(3):
                    bk = 4 + r
                    mm(PSF(bk, 0, 512), sel4[0:3, r, :], modrow[0:3, jb * 512:(jb + 1) * 512])
                    act("copy", out=gt[:, r, half * 512:(half + 1) * 512], in_=PSF(bk, 0, 512))
        dve("tensor_copy", out=modT, in_=v3(PSF(0, 0, 144), 48))
        for k, (jsc, gcol) in enumerate(((8, 0), (32, 8))):
            dve("tensor_scalar_add", out=scl[:, k], in0=modT[:, jsc:jsc + 8, :], scalar1=1.0)
            dve("tensor_tensor", out=scl[:, k], in0=scl[:, k],
                in1=colsL[:, gcol:gcol + 8].unsqueeze(2).to_broadcast([128, 8, 3]), op=ALU.mult)
        if dbg and l == 0:
            P.dma("sp", dbg_d["d_mod"], modT.rearrange("p a b -> p (a b)"))
            P.dma("sp", dbg_d["d_g1b"], g1b.rearrange("p a b -> p (a b)"))
        A.pop()
        P.barrier()
        if stop == "ada":
            break

        for b in range(2):
            A.push()
            hmT = A.alloc([8, NTOK], BF16)
            gate_all = A.alloc([16, 8], F32)
            A.push()
            mixAT = A.alloc([2, NTOK], BF16)
            mixCT = A.alloc([2, NTOK], BF16)
            tiles = list(range(NT))

            def src_tile(t):
                if t < 2:
                    base = cin if l == 0 else xc1_d
                    return base[b, t * 128:(t + 1) * 128, :]
                base = xin if l == 0 else x1_d
                return base[b, (t - 2) * 128:(t - 1) * 128, :]

            def norm_mod_T(xt, k, r, outT, tcols, f32path=False, hTf=None):
                ss = sm[:, 0:1]
                act("activation", out=junkb, in_=xt, func=AF.Square, accum_out=ss)
                rstd_from_ss(sm[:, 1:2], ss, D, sm[:, 2:3])
                shift_j = 0 if k == 0 else 24
                if not f32path:
                    xn = A_xn
                    dve("tensor_scalar_mul", out=xn, in0=xt, scalar1=sm[:, 1:2])
                    for kc in range(8):
                        pe("transpose", out=PSB(0, kc * 128, (kc + 1) * 128), in_=xn[:, kc * 128:(kc + 1) * 128], identity=identb)
                    for kc in range(8):
                        act("activation", out=outT[:, kc, tcols[0]:tcols[1]], in_=PSB(0, kc * 128, (kc + 1) * 128),
                            func=AF.Identity, scale=scl[:, k, kc, r:r + 1], bias=modT[:, shift_j + kc, r:r + 1])
                else:
                    xn = A_xnf
                    dve("tensor_scalar_mul", out=xn, in0=xt, scalar1=sm[:, 1:2])
                    for kc in range(8):
                        pe("transpose", out=PSF(2 + kc // 4, (kc % 4) * 128, (kc % 4 + 1) * 128),
                           in_=xn[:, kc * 128:(kc + 1) * 128], identity=identf)
                    for kc in range(8):
                        act("activation", out=hTf[:, kc, :], in_=PSF(2 + kc // 4, (kc % 4) * 128, (kc % 4 + 1) * 128),
                            func=AF.Identity, scale=scl[:, k, kc, r:r + 1], bias=modT[:, shift_j + kc, r:r + 1])
                    dve("tensor_copy", out=outT[:, :, tcols[0]:tcols[1]], in_=hTf)

            A.push()
            w_in_sb = A.alloc([8, INC], BF16)
            for kc in range(8):
                P.dma("pool", w_in_sb[:, kc, :], w_in[l][kc * 128:(kc + 1) * 128, :])
            w_uq_sb = A.alloc([2, 768], BF16)
            P.dma("pool", w_uq_sb, w_uq[l].rearrange("(c p) n -> p c n", p=128))
            w_ukv_sb = A.alloc([1024], BF16)
            P.dma("pool", w_ukv_sb, w_ukv[l])
            gws_sb = A.alloc([4, 128], BF16)
            P.dma("pool", gws_sb, v3(gws[l], 4))
            xbuf = [A.alloc([D], F32) for _ in range(2)]
            A_xn = A.alloc([D], BF16)
            xmT = A.alloc([8, 128], BF16)
            qk_bf = A.alloc([512], BF16)
            qkT_st = [A.alloc([512], BF16) for _ in range(2)]
            kv_st = [A.alloc([516], BF16) for _ in range(2)]
            og_st = [A.alloc([272], F32) for _ in range(2)]
            v1_st = [A.alloc([8, 65], BF16) for _ in range(2)]
            for i in range(2):
                dve("memset", ap=kv_st[i], constant=1.0)
                dve("memset", ap=v1_st[i], constant=1.0)
            gt = A.alloc([16], F32)
            ef = A.alloc([8], F32)
            cqn = A.alloc([256], BF16)
            cqT = A.alloc([2, 128], BF16)
            ckvn = A.alloc([128], BF16)
            ckvT = A.alloc([128], BF16)
            krs = A.alloc([32], F32)
            ga = A.alloc([512], F32)
            bst = A.alloc([int(nc.vector.BN_STATS_DIM)], F32)
            bag = A.alloc([int(nc.vector.BN_AGGR_DIM)], F32)
            vn_f = A.alloc([256], F32)
            vn_bf = A.alloc([256], BF16)
            cf = A.alloc([256], F32)
            c_bf = A.alloc([256], BF16)
            qs = A.alloc([768], F32)
            sq = A.alloc([768], F32)
            qn = A.alloc([8, 96], F32)
            rtmp = A.alloc([4, 8, 16], F32)
            qr_bf = A.alloc([8, 96], BF16)
            qT_st = [A.alloc([8, 128], BF16, parts=96) for _ in range(2)]
            kvs = A.alloc([8, 128], F32)
            kn = A.alloc([8, 96], F32)
            kr_bf = A.alloc([8, 96], BF16)
            kT_st = [A.alloc([8, 128], BF16, parts=96) for _ in range(2)]
            s8 = A.alloc([32], F32)
            s8k = A.alloc([32], F32)
            sqk = A.alloc([512], F32)
            rtmpk = A.alloc([4, 8, 16], F32)

            def rope_apply(src, dst, tl):
                cosb = ropet[:, tl, 0:16].unsqueeze(1).to_broadcast([128, 8, 16])
                sinb = ropet[:, tl, 16:32].unsqueeze(1).to_broadcast([128, 8, 16])
                t1 = src[:, :, 64:80]
                t2 = src[:, :, 80:96]
                dve("tensor_tensor", out=rtmp[:, 0], in0=t1, in1=cosb, op=ALU.mult)
                dve("tensor_tensor", out=rtmp[:, 1], in0=t2, in1=sinb, op=ALU.mult)
                dve("tensor_tensor", out=rtmp[:, 2], in0=t1, in1=sinb, op=ALU.mult)
                dve("tensor_tensor", out=rtmp[:, 3], in0=t2, in1=cosb, op=ALU.mult)
                dve("tensor_tensor", out=dst[:, :, 64:80], in0=rtmp[:, 0], in1=rtmp[:, 1], op=ALU.subtract)
                dve("tensor_tensor", out=dst[:, :, 80:96], in0=rtmp[:, 2], in1=rtmp[:, 3], op=ALU.add)
                dve("tensor_copy", out=dst[:, :, 0:64], in_=src[:, :, 0:64])

            zc0 = [0, 512, 1024, 1456]
            zc1 = [512, 1024, 1456, 1968]

            def front(t):
                r = 2 if t < 2 else b
                xt = xbuf[t % 2]
                P.dma(XQ, xt, src_tile(t))
                norm_mod_T(xt, 0, r, xmT, (0, 128))
                for kc in range(8):
                    for g in range(4):
                        mm(PSF(1 + g, 0, zc1[g] - zc0[g]), xmT[:, kc, :], w_in_sb[:, kc, zc0[g]:zc1[g]],
                           start=kc == 0, stop=kc == 7)

            def evac(t):
                pp = t % 2
                need_c = not (last and t < 2)
                act("copy", out=qk_bf, in_=PSF(1, 0, 512))
                dve("tensor_copy", out=v3(kv_st[pp][:, 256:516], 4)[:, :, 0:64], in_=v3(PSF(2, 0, 256), 4))
                act("activation", out=og_st[pp][:, 0:256], in_=PSF(2, 256, 512), func=AF.Exp, scale=-1.0)
                dve("tensor_tensor", out=gt, in0=PSF(3, 0, 16), in1=bcL[:, GB:GB + 16], op=ALU.add)
                act("activation", out=junkf, in_=PSF(3, 16, 272), func=AF.Square, accum_out=sm[:, 4:5])
                act("activation", out=junkf[:, 0:128], in_=PSF(3, 272, 400), func=AF.Square, accum_out=sm[:, 8:9])
                act("copy", out=krs, in_=PSF(3, 400, 432))
                rstd_from_ss(sm[:, 5:6], sm[:, 4:5], 256, sm[:, 6:7])
                rstd_from_ss(sm[:, 9:10], sm[:, 8:9], 128, sm[:, 10:11])
                dve("scalar_tensor_tensor", out=cqn, in0=PSF(3, 16, 272), scalar=sm[:, 5:6], in1=bcL[:, CQG:CQG + 256],
                    op0=ALU.mult, op1=ALU.mult)
                dve("scalar_tensor_tensor", out=ckvn, in0=PSF(3, 272, 400), scalar=sm[:, 9:10], in1=bcL[:, CKVG:CKVG + 128],
                    op0=ALU.mult, op1=ALU.mult)
                if need_c:
                    act("activation", out=ga, in_=PSF(4, 0, 512), func=AF.Gelu_apprx_tanh)

            def back(t):
                pp = t % 2
                need_q = not (last and t < 2)
                need_c = need_q

                def chain_m():
                    for i in range(4):
                        pe("transpose", out=PSB(5, i * 128, (i + 1) * 128), in_=qk_bf[:, i * 128:(i + 1) * 128], identity=identb)
                    yield
                    dve("tensor_copy", out=qkT_st[pp], in_=PSB(5, 0, 512))
                    P.dma("sp", ml_qk_d[t], qkT_st[pp], WK=[("mlqk", t)])
                    pool("tensor_copy", out=kv_st[pp][:, 0:256], in_=qk_bf[:, 256:512])
                    yield
                    dve("tensor_scalar_add", out=og_st[pp][:, 0:256], in0=og_st[pp][:, 0:256], scalar1=1.0)
                    act("activation", out=ef, in_=gt[:, 8:16], func=AF.Exp, scale=-1.0)
                    yield
                    dve("reciprocal", out=og_st[pp][:, 0:256], in_=og_st[pp][:, 0:256])
                    act("activation", out=ef, in_=ef, func=AF.Ln, bias=1.0)
                    P.dma("sp", ml_kv_d[t], kv_st[pp], WK=[("mlkv", t)])
                    pool("tensor_copy", out=og_st[pp][:, 256:264], in_=gt[:, 0:8])
                    yield
                    dve("tensor_scalar_mul", out=og_st[pp][:, 264:272], in0=ef, scalar1=-1.0)
                    P.dma("sp", ml_og_d[t], og_st[pp], WK=[("mlog", t)])
                    yield

                def chain_pre():
                    for c in range(2):
                        pe("transpose", out=PSB(5, 512 + c * 128, 640 + c * 128), in_=cqn[:, c * 128:(c + 1) * 128], identity=identb)
                    pe("transpose", out=PSB(5, 768, 896), in_=ckvn, identity=identb)
                    yield
                    dve("tensor_copy", out=cqT, in_=v3(PSB(5, 512, 768), 2))
                    dve("tensor_copy", out=ckvT, in_=PSB(5, 768, 896))
                    yield
                    if need_q:
                        for c in range(2):
                            mm(PSF(6, 0, 512), cqT[:, c, :], w_uq_sb[:, c, 0:512], start=c == 0, stop=c == 1)
                        for c in range(2):
                            mm(PSF(7, 0, 256), cqT[:, c, :], w_uq_sb[:, c, 512:768], start=c == 0, stop=c == 1)
                        yield
                        act("copy", out=qs[:, 0:512], in_=PSF(6, 0, 512))
                        act("copy", out=qs[:, 512:768], in_=PSF(7, 0, 256))
                        yield
                    mm(PSF(6, 0, 512), ckvT, w_ukv_sb[:, 0:512])
                    mm(PSF(7, 0, 512), ckvT, w_ukv_sb[:, 512:1024])
                    yield
                    act("copy", out=kvs[:, 0:4, :], in_=v3(PSF(6, 0, 512), 4))
                    act("copy", out=kvs[:, 4:8, :], in_=v3(PSF(7, 0, 512), 4))
                    yield

                def chain_g():
                    if not need_c:
                        return
                    dve("bn_stats", out=bst, in_=ga[:, 256:512])
                    dve("bn_aggr", out=bag, in_=bst)
                    yield
                    rstd_from_ss(sm[:, 13:14], bag[:, 1:2], 1.0, sm[:, 12:13])
                    yield
                    dve("tensor_scalar", out=vn_f, in0=ga[:, 256:512], scalar1=bag[:, 0:1], scalar2=sm[:, 13:14],
                        op0=ALU.subtract, op1=ALU.mult)
                    yield
                    dve("tensor_tensor", out=vn_f, in0=vn_f, in1=bcL[:, LNG:LNG + 256], op=ALU.mult)
                    yield
                    dve("tensor_tensor", out=vn_bf, in0=vn_f, in1=bcL[:, LNB:LNB + 256], op=ALU.add)
                    yield
                    for g in range(4):
                        mm(PSF(5, g * 64, (g + 1) * 64), gws_sb[:, g, :], vn_bf[:, g * 64:(g + 1) * 64])
                    yield
                    dve("tensor_tensor", out=v3(cf, 4), in0=v3(PSF(5, 0, 256), 4),
                        in1=colsL[:, 64:68].unsqueeze(2).to_broadcast([128, 4, 64]), op=ALU.add)
                    yield
                    dve("tensor_tensor", out=c_bf, in0=cf, in1=ga[:, 0:256], op=ALU.mult)
                    yield
                    for c in range(2):
                        pe("transpose", out=PSB(5, 512 + c * 128, 640 + c * 128), in_=c_bf[:, c * 128:(c + 1) * 128], identity=identb)
                    yield
                    dve("tensor_copy", out=mixCT[:, :, t * 128:(t + 1) * 128], in_=v3(PSB(5, 512, 768), 2))
                    yield

                def rope_gen(src, dst, tl, tmp):
                    cosb = ropet[:, tl, 0:16].unsqueeze(1).to_broadcast([128, 8, 16])
                    sinb = ropet[:, tl, 16:32].unsqueeze(1).to_broadcast([128, 8, 16])
                    t1 = src[:, :, 64:80]
                    t2 = src[:, :, 80:96]
                    dve("tensor_tensor", out=tmp[:, 0], in0=t1, in1=cosb, op=ALU.mult)
                    yield
                    dve("tensor_tensor", out=tmp[:, 1], in0=t2, in1=sinb, op=ALU.mult)
                    yield
                    dve("tensor_tensor", out=tmp[:, 2], in0=t1, in1=sinb, op=ALU.mult)
                    yield
                    dve("tensor_tensor", out=tmp[:, 3], in0=t2, in1=cosb, op=ALU.mult)
                    yield
                    dve("tensor_tensor", out=dst[:, :, 64:80], in0=tmp[:, 0], in1=tmp[:, 1], op=ALU.subtract)
                    yield
                    dve("tensor_tensor", out=dst[:, :, 80:96], in0=tmp[:, 2], in1=tmp[:, 3], op=ALU.add)
                    yield
                    dve("tensor_copy", out=dst[:, :, 0:64], in_=src[:, :, 0:64])
                    yield

                def chain_q():
                    if not need_q:
                        return
                    dve("tensor_tensor", out=sq, in0=qs, in1=qs, op=ALU.mult)
                    yield
                    dve("tensor_reduce", out=s8[:, 0:8], in_=v3(sq, 8), axis=AX.X, op=ALU.add)
                    yield
                    rstd_from_ss(s8[:, 8:16], s8[:, 0:8], 96, s8[:, 16:24])
                    yield
                    dve("tensor_tensor", out=qn, in0=v3(qs, 8), in1=s8[:, 8:16].unsqueeze(2).to_broadcast([128, 8, 96]), op=ALU.mult)
                    yield
                    if t >= 2:
                        dve("tensor_tensor", out=qn, in0=qn, in1=v3(bcL[:, QG:QG + 768], 8), op=ALU.mult)
                        yield
                        yield from rope_gen(qn, qr_bf, t - 2, rtmp)
                    else:
                        dve("tensor_tensor", out=qr_bf, in0=qn, in1=v3(bcL[:, QG:QG + 768], 8), op=ALU.mult)
                        yield
                    for h in range(8):
                        pe("transpose", out=PSB(6, h * 128, (h + 1) * 128, parts=96), in_=qr_bf[:, h, :], identity=identb)
                    yield
                    act("copy", out=qT_st[pp], in_=v3(PSB(6, 0, 1024, parts=96), 8))
                    P.dma("sp", qt_d[:, :, t * 128:(t + 1) * 128], qT_st[pp], WK=[("qt", t)])
                    yield

                def chain_k():
                    pool("tensor_copy", out=v1_st[pp][:, :, 0:64], in_=kvs[:, :, 64:128])
                    P.dma("sp", v1_d[t], v1_st[pp].rearrange("p a b -> p (a b)"), WK=[("v1", t)])
                    dve("tensor_tensor", out=v3(sqk, 8), in0=kvs[:, :, 0:64], in1=kvs[:, :, 0:64], op=ALU.mult)
                    act("activation", out=junkf[:, 0:32], in_=krs, func=AF.Square, accum_out=sm[:, 16:17])
                    yield
                    dve("tensor_reduce", out=s8k[:, 0:8], in_=v3(sqk, 8), axis=AX.X, op=ALU.add)
                    yield
                    dve("tensor_scalar_add", out=s8k[:, 0:8], in0=s8k[:, 0:8], scalar1=sm[:, 16:17])
                    yield
                    rstd_from_ss(s8k[:, 8:16], s8k[:, 0:8], 96, s8k[:, 16:24])
                    yield
                    rkb = s8k[:, 8:16]
                    dve("tensor_tensor", out=kn[:, :, 0:64], in0=kvs[:, :, 0:64], in1=rkb.unsqueeze(2).to_broadcast([128, 8, 64]), op=ALU.mult)
                    yield
                    dve("tensor_tensor", out=kn[:, :, 64:96], in0=krs.unsqueeze(1).to_broadcast([128, 8, 32]),
                        in1=rkb.unsqueeze(2).to_broadcast([128, 8, 32]), op=ALU.mult)
                    yield
                    if t >= 2:
                        dve("tensor_tensor", out=kn, in0=kn, in1=v3(bcL[:, KG:KG + 768], 8), op=ALU.mult)
                        yield
                        yield from rope_gen(kn, kr_bf, t - 2, rtmpk)
                    else:
                        dve("tensor_tensor", out=kr_bf, in0=kn, in1=v3(bcL[:, KG:KG + 768], 8), op=ALU.mult)
                        yield
                    for h in range(8):
                        pe("transpose", out=PSB(7, h * 128, (h + 1) * 128, parts=96), in_=kr_bf[:, h, :], identity=identb)
                    yield
                    act("copy", out=kT_st[pp], in_=v3(PSB(7, 0, 1024, parts=96), 8))
                    P.dma("sp", kt_d[:, :, t * 128:(t + 1) * 128], kT_st[pp], WK=[("kt", t)])
                    yield

                def rr(gens):
                    gens = list(gens)
                    while gens:
                        for g_ in list(gens):
                            try:
                                next(g_)
                            except StopIteration:
                                gens.remove(g_)

                gm, gg = chain_m(), chain_g()
                live = [gm, gg]
                gp = chain_pre()
                while True:
                    try:
                        next(gp)
                    except StopIteration:
                        break
                    for g_ in list(live):
                        try:
                            next(g_)
                        except StopIteration:
                            live.remove(g_)
                rr(live + [chain_q(), chain_k()])

            front(tiles[0])
            for idx, t in enumerate(tiles):
                evac(t)
                if idx + 1 < len(tiles):
                    front(tiles[idx + 1])
                back(t)
            A.pop()
            P.barrier()
            if dbg and l == 0 and b == 0:
                P.dma("sp", dbg_d["d_mixC"].bitcast(BF16)[:, 0:2 * NTOK], mixCT.rearrange("p a b -> p (a b)"))
            if stop == "A":
                break

            A.push()
            Cn_f = A.alloc([2, 2, 65], F32)
            Cn_bf = A.alloc([2, 2, 65], BF16)
            mst = A.alloc([8], F32)
            dve("memset", ap=Cn_f, constant=0.0)
            dve("memset", ap=Cn_bf, constant=0.0)
            dve("memset", ap=mst, constant=0.0)
            hsum = A.alloc([NT, 256], F32)
            NB_ = 2

            class VS:
                pass
            vs = []
            for d in range(2):
                v = VS()
                v.qkb = [A.alloc([512], BF16) for _ in range(NB_)]
                v.kvb = [A.alloc([516], BF16) for _ in range(NB_)]
                v.ogb = [A.alloc([272], F32) for _ in range(NB_)]
                v.bb = A.alloc([8], F32)
                v.u = A.alloc([4], F32)
                v.uT = A.alloc([128], F32, parts=4)
                v.M1T = A.alloc([128], F32, parts=4)
                v.tmpU = A.alloc([4, 128], F32)
                v.cm = A.alloc([4], F32)
                v.cml = A.alloc([4], F32)
                v.mx = A.alloc([4], F32)
                v.M1 = A.alloc([4], F32)
                v.E4 = A.alloc([16], F32)
                v.X4 = A.alloc([16], F32)
                v.ET = A.alloc([512], BF16)
                v.PT = A.alloc([512], BF16)
                v.t1 = A.alloc([4, 65], F32)
                v.tot = A.alloc([4, 65], F32)
                v.dd = A.alloc([4], F32)
                v.rec = A.alloc([4], F32)
                v.t2 = A.alloc([4, 64], F32)
                v.kw = A.alloc([4, 64], BF16)
                v.sqh = A.alloc([4, 64], F32)
                v.an = A.alloc([4, 64], F32)
                v.a_bf = A.alloc([256], BF16)
                v.fs = A.alloc([16], F32)
                v.qm = A.alloc([4, 128], BF16)
                v.B = 4 * d
                vs.append(v)
            fwd = list(range(NT))
            bwd = [1, 0] + list(range(NT - 1, 1, -1))
            visited = set()

            def stage1(v, d, t, step, with_h):
                bi = step % NB_
                v.qk, v.kv, v.og = v.qkb[bi], v.kvb[bi], v.ogb[bi]
                P.dma("sp", v.qk, ml_qk_d[t], RK=[("mlqk", t)])
                yield
                P.dma("act", v.kv, ml_kv_d[t], RK=[("mlkv", t)])
                yield
                P.dma("sp", v.og, ml_og_d[t], RK=[("mlog", t)])
                yield
                B0 = v.B
                f_ = v.og[:, 264 + 4 * d:268 + 4 * d]
                i_ = v.og[:, 256 + 4 * d:260 + 4 * d]
                mm(PSF(B0, 0, 4), tri[d], f_)
                yield
                mm(PSF(B0, 4, 8), ones_f, f_)
                yield
                dve("tensor_copy", out=v.bb, in_=PSF(B0, 0, 8))
                yield
                dve("tensor_tensor", out=v.u, in0=i_, in1=v.bb[:, 0:4], op=ALU.subtract)
                yield
                pe("transpose", out=PSF(B0, 16, 144, parts=4), in_=v.u, identity=identf)
                yield
                act("copy", out=v.uT, in_=PSF(B0, 16, 144, parts=4))
                yield
                for h in range(4):
                    mm(PSF(B0 + 1, h * 128, (h + 1) * 128), sel4[:, h, :], v.uT)
                    yield
                if with_h:
                    for half in range(2):
                        dve("tensor_scalar_mul", out=v.qm[:, half:4:2, :], in0=v3(v.qk[:, 0:256], 2), scalar1=hmask[:, half:half + 1])
                    for h in range(4):
                        mm(PSF(B0 + 3, h * 65, (h + 1) * 65), v.qm[:, h, :], Cn_bf[:, d, h // 2, :])

            def stage2(v, d, t, step, with_h):
                B0 = v.B
                md = mst[:, 4 * d:4 * d + 4]
                if with_h:
                    dve("tensor_tensor", out=v.tmpU, in0=v3(PSF(B0 + 1, 0, 512), 4),
                        in1=mask[1 - d].unsqueeze(1).to_broadcast([128, 4, 128]), op=ALU.add)
                    yield
                    dve("tensor_reduce", out=v.cm, in_=v.tmpU, axis=AX.X, op=ALU.max)
                    yield
                dve("tensor_reduce", out=v.cml, in_=v3(PSF(B0 + 1, 0, 512), 4), axis=AX.X, op=ALU.max)
                yield
                dve("tensor_tensor", out=v.mx, in0=md, in1=v.cml, op=ALU.max)
                yield
                dve("tensor_tensor", out=v.E4[:, 8:12], in0=md, in1=v.mx, op=ALU.subtract)
                yield
                dve("tensor_tensor", out=v.E4[:, 12:16], in0=v.u, in1=v.mx, op=ALU.subtract)
                yield
                if with_h:
                    dve("tensor_tensor", out=v.M1, in0=md, in1=v.cm, op=ALU.max)
                    yield
                    dve("tensor_tensor", out=v.E4[:, 0:4], in0=md, in1=v.M1, op=ALU.subtract)
                    yield
                    dve("scalar_tensor_tensor", out=v.E4[:, 4:8], in0=v.bb[:, 0:4], scalar=-1.0, in1=v.M1,
                        op0=ALU.mult, op1=ALU.subtract)
                    yield
                    act("activation", out=v.X4, in_=v.E4, func=AF.Exp)
                    yield
                    pe("transpose", out=PSF(B0, 144, 272, parts=4), in_=v.M1, identity=identf)
                    yield
                    act("copy", out=v.M1T, in_=PSF(B0, 144, 272, parts=4))
                    yield
                    for h in range(4):
                        o_ = PSF(B0 + 2, h * 128, (h + 1) * 128)
                        mm(o_, v.uT, sel4[:, h, :], start=True, stop=False)
                        mm(o_, sel4n[:, h, :], v.M1T, start=False, stop=False)
                        mm(o_, identf, mask[d], start=False, stop=True)
                    for h in range(4):
                        mm(PSF(B0 + 1, h * 128, (h + 1) * 128), v.qk[:, (2 + h // 2) * 128:(3 + h // 2) * 128], v.qm[:, h, :])
                else:
                    act("activation", out=v.X4[:, 8:16], in_=v.E4[:, 8:16], func=AF.Exp)
                    yield
                dve("tensor_tensor", out=md, in0=v.bb[:, 4:8], in1=v.mx, op=ALU.add)
                yield

            def stage3(v, d, t, step, with_h):
                B0 = v.B
                if not with_h:
                    return
                a_, emt = v.X4[:, 0:4], v.X4[:, 4:8]
                v1 = v3(v.kv[:, 256:516], 4)
                act("activation", out=v.ET, in_=PSF(B0 + 2, 0, 512), func=AF.Exp)
                yield
                dve("scalar_tensor_tensor", out=v.PT, in0=PSF(B0 + 1, 0, 512), scalar=0.125, in1=v.ET, op0=ALU.mult, op1=ALU.mult)
                yield
                for h in range(4):
                    mm(PSF(B0 + 2, h * 65, (h + 1) * 65), v.PT[:, h * 128:(h + 1) * 128], v1[:, h, :])
                    yield
                dve("tensor_tensor", out=v.t1, in0=v3(PSF(B0 + 3, 0, 260), 4), in1=a_.unsqueeze(2).to_broadcast([128, 4, 65]), op=ALU.mult)
                yield
                dve("tensor_tensor", out=v.tot, in0=v3(PSF(B0 + 2, 0, 260), 4), in1=v.t1, op=ALU.add)
                yield
                den = v.tot[:, :, 64]
                dve("scalar_tensor_tensor", out=v.dd, in0=den, scalar=-1.0, in1=den, op0=ALU.mult, op1=ALU.max)
                yield
                dve("tensor_tensor", out=v.dd, in0=v.dd, in1=emt, op=ALU.max)
                yield
                dve("reciprocal", out=v.rec, in_=v.dd)
                yield
                hs = v3(hsum[:, t, :], 4)
                recb = v.rec.unsqueeze(2).to_broadcast([128, 4, 64])
                if t not in visited:
                    dve("tensor_tensor", out=hs, in0=v.tot[:, :, 0:64], in1=recb, op=ALU.mult)
                    yield
                else:
                    dve("tensor_tensor", out=v.t2, in0=v.tot[:, :, 0:64], in1=recb, op=ALU.mult)
                    yield
                    dve("tensor_tensor", out=hs, in0=hs, in1=v.t2, op=ALU.add)
                    yield
                    dve("tensor_tensor", out=v.sqh, in0=hs, in1=hs, op=ALU.mult)
                    yield
                    dve("tensor_reduce", out=v.fs[:, 0:4], in_=v.sqh, axis=AX.X, op=ALU.add)
                    yield
                    rstd_from_ss(v.fs[:, 4:8], v.fs[:, 0:4], 64, v.fs[:, 8:12])
                    yield
                    dve("tensor_tensor", out=v.an, in0=hs, in1=v.fs[:, 4:8].unsqueeze(2).to_broadcast([128, 4, 64]), op=ALU.mult)
                    yield
                    dve("tensor_tensor", out=v.an, in0=v.an, in1=v3(bcL[:, NG:NG + 256], 4), op=ALU.mult)
                    yield
                    dve("tensor_tensor", out=v3(v.a_bf, 4), in0=v.an, in1=v3(v.og[:, 0:256], 4), op=ALU.mult)
                    yield
                    for c in range(2):
                        pe("transpose", out=PSB(B0, 768 + c * 128, 896 + c * 128), in_=v.a_bf[:, c * 128:(c + 1) * 128], identity=identb)
                    dve("tensor_copy", out=mixAT[:, :, t * 128:(t + 1) * 128], in_=v3(PSB(B0, 768, 1024), 2))
                    yield
                visited.add(t)

            def stage4(v, d, t, step, with_h):
                B0 = v.B
                w_ = v.X4[:, 12:16]
                k3 = v3(v.kv[:, 0:256], 4)
                v1 = v3(v.kv[:, 256:516], 4)
                dve("scalar_tensor_tensor", out=v.kw, in0=k3, scalar=0.125, in1=w_.unsqueeze(2).to_broadcast([128, 4, 64]),
                    op0=ALU.mult, op1=ALU.mult)
                yield
                kwf = v.kw.rearrange("p a b -> p (a b)")
                for h in range(4):
                    mm(PSF(B0 + 1, h * 65, (h + 1) * 65), kwf[:, (h // 2) * 128:(h // 2 + 1) * 128], v1[:, h, :])
                    yield
                for half in range(2):
                    p0 = half * 64
                    psv = v3(PSF(B0 + 1, 0, 260, parts=64, p0=p0), 4)[:, half:4:2, :]
                    cst = Cn_f[p0:p0 + 64, d]
                    dve("tensor_tensor", out=cst, in0=cst,
                        in1=v.X4[p0:p0 + 64, 8 + half:12:2].unsqueeze(2).to_broadcast([64, 2, 65]), op=ALU.mult)
                    yield
                    dve("tensor_tensor", out=cst, in0=cst, in1=psv, op=ALU.add)
                    yield
                    act("copy", out=Cn_bf[p0:p0 + 64, d], in_=cst)
                    yield

            def visit_gen(v, d, t, step, with_h):
                for stg in (stage1, stage2, stage3, stage4):
                    g_ = stg(v, d, t, step, with_h)
                    if g_ is not None:
                        yield from g_

            for step in range(NT):
                gens = []
                for d in range(2):
                    t = (fwd if d == 0 else bwd)[step]
                    gens.append(visit_gen(vs[d], d, t, step, not (last and t < 2)))
                while gens:
                    for g_ in list(gens):
                        try:
                            next(g_)
                        except StopIteration:
                            gens.remove(g_)
            A.pop()
            if dbg and l == 0 and b == 0:
                P.dma("sp", dbg_d["d_mixA"].bitcast(BF16)[:, 0:2 * NTOK], mixAT.rearrange("p a b -> p (a b)"))
            if stop == "B":
                break

            mixBT = A.alloc([8, NTOK], BF16, parts=64)
            A.push()
            QTb = [A.alloc([NTOK], BF16, parts=96) for _ in range(2)]
            KTb = [A.alloc([NTOK], BF16, parts=96) for _ in range(2)]
            V1 = A.alloc([NT, 520], BF16)
            P.dma("sp", V1, v1_d.rearrange("t p f -> p t f"), RK=[("v1", t_) for t_ in range(NT)])
            PTb = [A.alloc([512], BF16) for _ in range(4)]
            SBK = [0, 1, 2, 7]
            Os = [A.alloc([512], F32, parts=65) for _ in range(2)]
            recr = A.alloc([512], F32)
            scl_att = 96.0 ** -0.5
            blocks = []
            if not last:
                blocks.append((0, 256, [0, 1]))
            for qb in range(4):
                blocks.append((256 + qb * 512, 512, list(range(NT))))
            scnt = 0
            ocnt = 0
            pending = [None]

            def make_tail(h, q0, nq, bankO, bankB, osb):
                def tail():
                    dve("tensor_copy", out=osb[:, 0:nq], in_=PSF(bankO, 0, nq, parts=65))
                    dve("reciprocal", out=recr[64:65, 0:nq], in_=osb[64:65, 0:nq])
                    mm(PSF(bankB, 0, nq, parts=64), ones_f[64:65, 0:64], recr[64:65, 0:nq])
                    dve("tensor_tensor", out=mixBT[:, h, q0:q0 + nq], in0=osb[0:64, 0:nq], in1=PSF(bankB, 0, nq, parts=64), op=ALU.mult)
                return tail

            items = []
            oc = 0
            for h in range(8):
                for (q0, nq, kts) in blocks:
                    for i, kt in enumerate(kts):
                        items.append((h, q0, nq, kt, i, len(kts), oc))
                    oc += 1
            loaded = set()

            def load_head(h):
                if h < 8 and h not in loaded:
                    loaded.add(h)
                    P.dma("sp", QTb[h % 2], qt_d[:, h, :], RK=[("qt", t_) for t_ in range(NT)])
                    P.dma("act", KTb[h % 2], kt_d[:, h, :], RK=[("kt", t_) for t_ in range(NT)])

            def S_item(j):
                h, q0, nq, kt, i, nk, oc_ = items[j]
                load_head(h)
                mm(PSF(SBK[j % 4], 0, nq), KTb[h % 2][:, kt * 128:(kt + 1) * 128], QTb[h % 2][:, q0:q0 + nq])

            NI = len(items)
            for j in range(min(3, NI)):
                S_item(j)
            for j, (h, q0, nq, kt, i, nk, oc_) in enumerate(items):
                bankO = 3 + oc_ % 2
                bankB = 5 + oc_ % 2
                osb = Os[oc_ % 2]
                pb = PTb[j % 4]
                act("activation", out=pb[:, 0:nq], in_=PSF(SBK[j % 4], 0, nq), func=AF.Exp, scale=scl_att)
                mm(PSF(bankO, 0, nq, parts=65), V1[:, kt, h * 65:(h + 1) * 65], pb[:, 0:nq], start=i == 0, stop=i == nk - 1)
                if j + 3 < NI:
                    S_item(j + 3)
                if i == min(1, nk - 1) and pending[0] is not None:
                    pending[0]()
                    pending[0] = None
                if i == nk - 1:
                    if pending[0] is not None:
                        pending[0]()
                    pending[0] = make_tail(h, q0, nq, bankO, bankB, osb)
            if pending[0] is not None:
                pending[0]()
            A.pop()
            if dbg and l == 0 and b == 0:
                P.dma("sp", dbg_d["d_mixB"].bitcast(BF16)[:, 0:8 * NTOK], mixBT.rearrange("p a b -> p (a b)"))
            if stop == "C":
                break

            A.push()
            woA = A.alloc([2, D], BF16)
            woB = A.alloc([8, D], BF16, parts=64)
            woC = A.alloc([2, D], BF16)
            P.dma("pool", woA, w_out[l][0:256, :].rearrange("(c p) n -> p c n", p=128))
            P.dma("pool", woB, w_out[l][256:768, :].rearrange("(h p) n -> p h n", p=64))
            P.dma("pool", woC, w_out[l][768:1024, :].rearrange("(c p) n -> p c n", p=128))
            xbuf = [A.alloc([D], F32) for _ in range(2)]
            xmb = [A.alloc([D], F32) for _ in range(2)]
            A_xn = A.alloc([D], BF16)
            if last:
                A_xnf = A.alloc([D], F32)
                hTf = A.alloc([8, 128], F32)
                rw_sb = A.alloc([8, 8], F32)
                P.dma("sp", rw_sb, v3(rw_d, 8))
                lg = A.alloc([8], F32)
                l2 = A.alloc([8], F32)
                eq1 = A.alloc([8], F32)
                eq2 = A.alloc([8], F32)
                tp = A.alloc([16], F32)
            d1_tiles = [t for t in tiles if not (last and t < 2)]

            def d1_mm(t):
                pp = t % 2
                P.dma("sp", xbuf[pp], src_tile(t))
                tc0, tc1 = t * 128, (t + 1) * 128
                for cg in range(2):
                    o_ = PSF(4 + 2 * pp + cg, 0, 512)
                    cs = slice(cg * 512, (cg + 1) * 512)
                    chunks = [(mixAT[:, 0, tc0:tc1], woA[:, 0, cs]), (mixAT[:, 1, tc0:tc1], woA[:, 1, cs])]
                    chunks += [(mixBT[:, h, tc0:tc1], woB[:, h, cs]) for h in range(8)]
                    chunks += [(mixCT[:, 0, tc0:tc1], woC[:, 0, cs]), (mixCT[:, 1, tc0:tc1], woC[:, 1, cs])]
                    for i, (lt, rh) in enumerate(chunks):
                        mm(o_, lt, rh, start=i == 0, stop=i == len(chunks) - 1)

            def d1_evac(t):
                r = 2 if t < 2 else b
                pp = t % 2
                xt, xm = xbuf[pp], xmb[pp]
                dve("tensor_tensor", out=xm, in0=ps[:, (4 + 2 * pp) * 512:(6 + 2 * pp) * 512], in1=g1b[:, r, :], op=ALU.mult)
                dve("tensor_tensor", out=xm, in0=xm, in1=xt, op=ALU.add)
                P.dma("sp", xmid_d[t], xm, WK=[("xmid", t)])
                if dbg and l == 0 and b == 0:
                    P.dma("sp", dbg_d["d_xmid"][t * 128:(t + 1) * 128, :], xm)

            def d1_norm(t):
                r = 2 if t < 2 else b
                xm = xmb[t % 2]
                tc0, tc1 = t * 128, (t + 1) * 128
                if not last:
                    norm_mod_T(xm, 1, r, hmT, (tc0, tc1))
                else:
                    norm_mod_T(xm, 1, r, hmT, (tc0, tc1), f32path=True, hTf=hTf)
                    for kc in range(8):
                        mm(PSF(1, 0, 8), hTf[:, kc, :], rw_sb[:, kc, :], start=kc == 0, stop=kc == 7)
                    dve("tensor_tensor", out=lg, in0=PSF(1, 0, 8), in1=bcL[:, RB:RB + 8], op=ALU.add)
                    dve("tensor_reduce", out=tp[:, 0:1], in_=lg, axis=AX.X, op=ALU.max)
                    dve("tensor_tensor", out=eq1, in0=lg, in1=tp[:, 0:1].to_broadcast([128, 8]), op=ALU.is_equal)
                    dve("scalar_tensor_tensor", out=l2, in0=eq1, scalar=-1e30, in1=lg, op0=ALU.mult, op1=ALU.add)
                    dve("tensor_reduce", out=tp[:, 1:2], in_=l2, axis=AX.X, op=ALU.max)
                    dve("tensor_tensor", out=eq2, in0=l2, in1=tp[:, 1:2].to_broadcast([128, 8]), op=ALU.is_equal)
                    dve("tensor_tensor", out=tp[:, 2:3], in0=tp[:, 1:2], in1=tp[:, 0:1], op=ALU.subtract)
                    act("activation", out=tp[:, 3:4], in_=tp[:, 2:3], func=AF.Exp)
                    dve("tensor_scalar_add", out=tp[:, 4:5], in0=tp[:, 3:4], scalar1=1.0)
                    dve("reciprocal", out=tp[:, 5:6], in_=tp[:, 4:5])
                    dve("tensor_tensor", out=tp[:, 6:7], in0=tp[:, 3:4], in1=tp[:, 5:6], op=ALU.mult)
                    gsl = gate_all[:, t - 2, :]
                    dve("tensor_scalar_mul", out=gsl, in0=eq1, scalar1=tp[:, 5:6])
                    dve("scalar_tensor_tensor", out=gsl, in0=eq2, scalar=tp[:, 6:7], in1=gsl, op0=ALU.mult, op1=ALU.add)

            d1_mm(d1_tiles[0])
            for i_, t in enumerate(d1_tiles):
                d1_evac(t)
                if i_ + 1 < len(d1_tiles):
                    d1_mm(d1_tiles[i_ + 1])
                d1_norm(t)
            A.pop()
            A.pop()
            P.barrier()
            if stop == "D1":
                break

            A.push()
            y_acc = A.alloc([NT, D], F32)
            G = 2
            if not last:
                experts = [None]
                nff = DFF // 128
                tok_blocks = [(0, 512), (512, 512), (1024, 512), (1536, 512), (2048, 256)]
            else:
                experts = list(range(8)) if with_moe else []
                nff = DFE // 128
                tok_blocks = [(256 + i * 512, 512) for i in range(4)]
            A.push()
            w1b = [A.alloc([8, G * 128], BF16) for _ in range(2)]
            w3b = [A.alloc([8, G * 128], BF16) for _ in range(2)]
            w2b = [A.alloc([G, D], BF16) for _ in range(2)]
            gTb = [A.alloc([G, NTOK], BF16) for _ in range(2)]
            s1b = [A.alloc([512], BF16) for _ in range(2)]
            groups = []
            for e in experts:
                for gi in range(nff // G):
                    groups.append((e, gi))
            cnt = {"b": 0, "y": 0}

            def up_gen(gidx):
                e, gi = groups[gidx]
                if e is None:
                    W1, W3, W2 = ffn_w1, ffn_w3, ffn_w2
                else:
                    W1, W3, W2 = moe_w1[e], moe_w3[e], moe_w2[e]
                w1g, w3g, w2g, gT = w1b[gidx % 2], w3b[gidx % 2], w2b[gidx % 2], gTb[gidx % 2]
                c0, c1 = gi * G * 128, (gi + 1) * G * 128
                P.dma("pool", w1g, W1[:, c0:c1].rearrange("(kc p) n -> p kc n", p=128))
                P.dma("pool", w3g, W3[:, c0:c1].rearrange("(kc p) n -> p kc n", p=128))
                P.dma("pool", w2g, W2[c0:c1, :].rearrange("(f p) n -> p f n", p=128))
                for f in range(G):
                    for (q0, n) in tok_blocks:
                        bk = cnt["b"] % 2
                        cnt["b"] += 1
                        for kc in range(8):
                            mm(PSF(bk, 0, n), w1g[:, kc, f * 128:(f + 1) * 128], hmT[:, kc, q0:q0 + n], start=kc == 0, stop=kc == 7)
                        for kc in range(8):
                            mm(PSF(2 + bk, 0, n), w3g[:, kc, f * 128:(f + 1) * 128], hmT[:, kc, q0:q0 + n], start=kc == 0, stop=kc == 7)
                        s1 = s1b[bk]
                        act("activation", out=s1[:, 0:n], in_=PSF(bk, 0, n), func=AF.Silu)
                        dve("tensor_tensor", out=gT[:, f, q0:q0 + n], in0=PSF(2 + bk, 0, n), in1=s1[:, 0:n], op=ALU.mult)
                        yield

            def down_gen(gidx):
                e, gi = groups[gidx]
                w2g, gT = w2b[gidx % 2], gTb[gidx % 2]
                first = (gidx == 0)
                for ti, t in enumerate(d1_tiles):
                    by = 4 + 2 * (cnt["y"] % 2)
                    cnt["y"] += 1
                    for cg in range(2):
                        for f in range(G):
                            mm(PSF(by + cg, 0, 512), gT[:, f, t * 128:(t + 1) * 128], w2g[:, f, cg * 512:(cg + 1) * 512],
                               start=f == 0, stop=f == G - 1)
                    ya = y_acc[:, t, :]
                    psy = ps[:, by * 512:(by + 2) * 512]
                    if e is None:
                        if first:
                            act("copy", out=ya, in_=psy)
                        else:
                            dve("tensor_tensor", out=ya, in0=psy, in1=ya, op=ALU.add)
                    else:
                        gsc = gate_all[:, t - 2, e:e + 1]
                        if first:
                            dve("tensor_scalar_mul", out=ya, in0=psy, scalar1=gsc)
                        else:
                            dve("scalar_tensor_tensor", out=ya, in0=psy, scalar=gsc, in1=ya,
                                op0=ALU.mult, op1=ALU.add)
                    if ti % 2 == 1:
                        yield

            def run_rr(gens):
                gens = [g_ for g_ in gens if g_ is not None]
                while gens:
                    for g_ in list(gens):
                        try:
                            next(g_)
                        except StopIteration:
                            gens.remove(g_)

            if groups:
                run_rr([up_gen(0)])
                for gidx in range(len(groups)):
                    nxt = up_gen(gidx + 1) if gidx + 1 < len(groups) else None
                    run_rr([nxt, down_gen(gidx)])
            A.pop()
            xbuf = [A.alloc([D], F32) for _ in range(2)]
            for t in d1_tiles:
                r = 2 if t < 2 else b
                xt = xbuf[t % 2]
                P.dma("sp", xt, xmid_d[t], RK=[("xmid", t)])
                if experts:
                    dve("tensor_tensor", out=y_acc[:, t, :], in0=y_acc[:, t, :], in1=g2b[:, r, :], op=ALU.mult)
                    dve("tensor_tensor", out=xt, in0=xt, in1=y_acc[:, t, :], op=ALU.add)
                if last:
                    dst = out_d[b, (t - 2) * 128:(t - 1) * 128, :]
                elif t < 2:
                    dst = xc1_d[b, t * 128:(t + 1) * 128, :]
                else:
                    dst = x1_d[b, (t - 2) * 128:(t - 1) * 128, :]
                P.dma("act", dst, xt)
            A.pop()
            P.barrier()
            A.pop()
        else:
            A.pop()
            continue
        break
    P.emit()
    print("ops", P.stats, "sbuf peak", A.peak)
    return nc, es


def host_consts():
    idn = np.eye(128, dtype=np.float32)
    s = np.arange(128)[:, None]
    t = np.arange(128)[None, :]
    triF = (s <= t).astype(np.float32)
    triB = (s >= t).astype(np.float32)
    maskF = np.where(s <= t, 0.0, NEG).astype(np.float32)
    maskB = np.where(s >= t, 0.0, NEG).astype(np.float32)
    ones = np.ones((128, 128), np.float32)
    hm = np.zeros((128, 2), np.float32)
    hm[:64, 0] = 1.0
    hm[64:, 1] = 1.0
    consts = np.concatenate([idn, triF, triB, maskF, maskB, ones, hm], axis=1)
    sel = np.zeros((4, 2, 4, 128), np.float32)
    for h in range(4):
        sel[h, 0, h, :] = 1.0
        sel[h, 1, h, :] = -1.0
    sel = sel.reshape(4, 1024)
    rows = S // 64
    row = np.repeat(np.arange(rows), 64).astype(np.float32)
    col = np.tile(np.arange(64), rows).astype(np.float32)
    inv = (np.float32(10000.0) ** (-np.arange(8, dtype=np.float32) / np.float32(8))).astype(np.float32)
    ang = np.concatenate([row[:, None] * inv, col[:, None] * inv], axis=-1).astype(np.float32)
    cs = np.concatenate([np.cos(ang), np.sin(ang)], axis=-1).astype(np.float32)
    rope = cs.reshape(16, 128, 32).transpose(1, 0, 2).reshape(128, 512)
    return consts, sel, np.ascontiguousarray(rope)


def make_in_maps(inp, ncores=8, with_moe=True):
    consts, sel, rope = host_consts()
    f = lambda a: np.ascontiguousarray(np.asarray(a, dtype=np.float32))
    L = 2
    bc = np.zeros((L, 128, NBC), np.float32)
    cols = np.zeros((L, 128, NCOLS), np.float32)
    for l in range(L):
        row = np.concatenate([
            inp["mlstm_norm_g"][l], inp["mla_cq_g"][l], inp["mla_ckv_g"][l],
            np.tile(inp["mla_q_g"][l], 8), np.tile(inp["mla_k_g"][l], 8),
            inp["gmlp_ln_g"][l], inp["gmlp_ln_b"][l], inp["mlstm_gate_b"][l],
            inp["moe_router_b"][0] if l == 1 else np.zeros(8, np.float32)]).astype(np.float32)
        assert row.shape[0] == NBC
        bc[l] = np.broadcast_to(row[None, :], (128, NBC))
        cols[l, :, 0:8] = inp["norm1_g"][l].reshape(8, 128).T
        cols[l, :, 8:16] = inp["norm2_g"][l].reshape(8, 128).T
        cols[l, :, 16:64] = inp["ada_b"][l].reshape(48, 128).T
        cols[l, :, 64:68] = inp["gmlp_b_s"][l].T
    gws = np.ascontiguousarray(np.transpose(inp["gmlp_w_s"], (0, 3, 1, 2))).reshape(L, 128, 512)
    rows = f(inp["ada_b"]).reshape(L, 1, 6 * D)
    rw = f(inp["moe_router_w"][0]).reshape(8, 128, 8).transpose(1, 0, 2).reshape(128, 64)
    shared = {
        "consts": consts, "sel": sel, "rope": rope,
        "ada_w": f(inp["ada_w"]), "w_in": f(inp["w_in"]), "w_out": f(inp["w_out"]),
        "w_uq": f(inp["mla_w_uq"]), "w_ukv": f(inp["mla_w_ukv"]), "gws": f(gws),
        "bc": bc, "cols": cols, "rows": rows,
        "ffn_w1": f(inp["ffn_w1"][0]), "ffn_w3": f(inp["ffn_w3"][0]), "ffn_w2": f(inp["ffn_w2"][0]),
    }
    if with_moe:
        shared.update({"moe_w1": f(inp["moe_w1"][0]), "moe_w3": f(inp["moe_w3"][0]), "moe_w2": f(inp["moe_w2"][0]),
                       "rw": np.ascontiguousarray(rw)})
    maps = []
    for c in range(ncores):
        cc = np.stack([inp["c"][2 * c], inp["c"][2 * c + 1], inp["c_ctx"]]).astype(np.float32)
        cT = cc.reshape(3, 8, 128).transpose(2, 1, 0).reshape(128, 24)
        m = dict(shared)
        m["xin"] = f(inp["x"][2 * c:2 * c + 2])
        m["cin"] = f(inp["ctx"][2 * c:2 * c + 2])
        m["cT"] = np.ascontiguousarray(cT)
        maps.append(m)
    return maps


def kernel(**inp):
    nc, es = build()
    maps = make_in_maps(inp)
    res = run_bass_kernel_spmd(nc, maps, core_ids=list(range(8)))
    es.close()
    return np.concatenate([r["out"] for r in res.results], axis=0)
```

```python
from collections import defaultdict
from contextlib import ExitStack
import concourse.bass as bass
import concourse.mybir as mybir

F32 = mybir.dt.float32
BF16 = mybir.dt.bfloat16
U8 = mybir.dt.uint8
I32 = mybir.dt.int32
AF = mybir.ActivationFunctionType
ALU = mybir.AluOpType
AX = mybir.AxisListType
ESZ = {F32: 4, BF16: 2, U8: 1, I32: 4}

ENGS = ["pe", "act", "dve", "pool", "sp"]
WRITE_KW = ("out", "accum_out", "ap")
GRAN = 512
SEM_MAX = 30000
DO_CLEAR = False


def is_ap(v):
    return hasattr(v, "ap") and hasattr(v, "offset") and hasattr(v, "tensor")


def ap_box(ap):
    t = ap.tensor
    tn = type(t).__name__
    if tn.startswith("SB"):
        space = "sb:" + t.name
    elif tn.startswith("PSum"):
        space = "ps:" + t.name
    else:
        return None
    pairs = [tuple(p) for p in ap.ap]
    es = ESZ[ap.dtype]
    rowlen = int(t.shape[1]) if len(t.shape) == 2 else None
    pstep, pcnt = pairs[0]
    if pstep == 0:
        pstep = rowlen
    assert pstep == rowlen, (pstep, rowlen, ap)
    off = int(ap.offset)
    p0 = off // rowlen
    c0 = off % rowlen
    ext = 0
    for st, cn in pairs[1:]:
        ext += abs(int(st)) * (int(cn) - 1)
    b0 = c0 * es
    b1 = (c0 + ext + 1) * es
    return (space, p0, p0 + int(pcnt), b0, b1)


class Op:
    __slots__ = ("eng", "fn", "waits", "tok", "dsem", "kind", "arg")

    def __init__(self, eng, fn, kind="ins"):
        self.eng = eng
        self.fn = fn
        self.waits = []
        self.tok = None
        self.dsem = None
        self.kind = kind
        self.arg = None


class Prog:
    def __init__(self, nc, n_esem=40, n_dsem=48, same_eng_sync=True):
        self.nc = nc
        self.ops = {e: [] for e in ENGS}
        self.seen = {e: {} for e in ENGS}
        self.same = same_eng_sync
        self.buckets = defaultdict(lambda: defaultdict(list))
        self.rdedupe = {}
        self.psum = defaultdict(dict)
        self.keys = {}
        self.n_esem = n_esem
        self.n_dsem = n_dsem
        self.dcount = [0] * n_dsem
        self.dnext = 0
        self.epoch = 0
        self.ep_bounds = {e: [0] for e in ENGS}
        self.n_ops = 0

    def _need(self, E, tok, deps):
        if tok is None:
            return
        if tok[0] == "e":
            if tok[2] < self.ep_bounds[tok[1]][-1]:
                return
            if tok[1] == E and (E == "pe" or E == "sp" or not self.same):
                return
            k = ("e", tok[1])
            v = tok[2]
        else:
            if tok[1] < self.epoch:
                return
            k = ("d", tok[2])
            v = tok[3]
        if self.seen[E].get(k, -1) >= v:
            return
        if deps.get(k, -1) < v:
            deps[k] = v

    def _track_sb(self, E, box, is_w, deps, newrecs):
        space, p0, p1, b0, b1 = box
        bk = self.buckets[space]
        for bi in range(b0 // GRAN, (b1 - 1) // GRAN + 1):
            lst = bk[bi]
            keep = []
            for rec in lst:
                if not rec[6]:
                    continue
                if rec[0] < p1 and p0 < rec[1] and rec[2] < b1 and b0 < rec[3]:
                    if is_w or rec[4]:
                        self._need(E, rec[5], deps)
                    if is_w and p0 <= rec[0] and rec[1] <= p1 and b0 <= rec[2] and rec[3] <= b1:
                        rec[6] = False
                        continue
                keep.append(rec)
            bk[bi] = keep
        newrecs.append((box, is_w))

    def _commit_sb(self, E, tok, newrecs):
        for box, is_w in newrecs:
            space, p0, p1, b0, b1 = box
            if not is_w and tok[0] == "e":
                key = (box, E)
                old = self.rdedupe.get(key)
                if old is not None and old[6]:
                    old[5] = tok
                    continue
            rec = [p0, p1, b0, b1, is_w, tok, True]
            if not is_w and tok[0] == "e":
                self.rdedupe[(box, E)] = rec
            bk = self.buckets[space]
            for bi in range(b0 // GRAN, (b1 - 1) // GRAN + 1):
                bk[bi].append(rec)

    def _deps_for(self, E, R, W, RK, WK):
        deps = {}
        newrecs = []
        psb = []
        for ap, is_w in [(a, False) for a in R] + [(a, True) for a in W]:
            box = ap_box(ap)
            if box is None:
                continue
            if box[0].startswith("ps:"):
                for bank in range(box[3] // 2048, (box[4] - 1) // 2048 + 1):
                    key = (box[0], bank)
                    for F, tok in self.psum[key].items():
                        if F != E:
                            self._need(E, tok, deps)
                    psb.append(key)
            else:
                self._track_sb(E, box, is_w, deps, newrecs)
        for k in RK:
            st = self.keys.setdefault(k, {"w": None, "r": {}})
            self._need(E, st["w"], deps)
        for k in WK:
            st = self.keys.setdefault(k, {"w": None, "r": {}})
            self._need(E, st["w"], deps)
            for t in st["r"].values():
                self._need(E, t, deps)
        return deps, newrecs, psb

    def _finish(self, E, op, tok, deps, newrecs, psb, RK, WK):
        for k, v in deps.items():
            self.seen[E][k] = v
            op.waits.append((k, v, self.epoch))
        self._commit_sb(E, tok, newrecs)
        for key in psb:
            self.psum[key] = {E: tok}
        for k in RK:
            rk = E if tok[0] == "e" else ("d", tok[2])
            self.keys[k]["r"][rk] = tok
        for k in WK:
            self.keys[k]["w"] = tok
            self.keys[k]["r"] = {}
        self.ops[E].append(op)
        self.n_ops += 1

    def I(self, E, meth, RK=(), WK=(), **kw):
        R = [v for k, v in kw.items() if is_ap(v) and k not in WRITE_KW]
        W = [v for k, v in kw.items() if is_ap(v) and k in WRITE_KW]
        deps, newrecs, psb = self._deps_for(E, R, W, RK, WK)
        op = Op(E, lambda e: getattr(e, meth)(**kw))
        tok = ("e", E, len(self.ops[E]))
        op.tok = tok
        self._finish(E, op, tok, deps, newrecs, psb, RK, WK)
        return op

    def dma(self, Q, out, in_, RK=(), WK=(), **kw):
        i = self.dnext
        self.dnext = (self.dnext + 1) % self.n_dsem
        deps, newrecs, psb = self._deps_for(Q, [in_], [out], RK, WK)
        if self.dcount[i] > 0:
            self._need(Q, ("d", self.epoch, i, 16 * self.dcount[i]), deps)
        self.dcount[i] += 1
        tok = ("d", self.epoch, i, 16 * self.dcount[i])
        op = Op(Q, lambda e: e.dma_start(out=out, in_=in_, **kw))
        op.dsem = (self.epoch, i)
        op.tok = tok
        self._finish(Q, op, tok, deps, newrecs, psb, RK, WK)
        return op

    def barrier(self):
        toks = []
        for e in ENGS:
            lo = self.ep_bounds[e][-1]
            for idx in range(len(self.ops[e]) - 1, lo - 1, -1):
                t = self.ops[e][idx].tok
                if t is not None and t[0] == "e":
                    toks.append(t)
                    break
        for i in range(self.n_dsem):
            if self.dcount[i]:
                toks.append(("d", self.epoch, i, 16 * self.dcount[i]))
        bops = []
        for e in ENGS:
            deps = {}
            for t in toks:
                if t[0] == "e" and t[1] == e:
                    continue
                self._need(e, t, deps)
            op = Op(e, None, kind="barrier")
            op.waits = [(k, v, self.epoch) for k, v in deps.items()]
            bops.append(op)
        self.epoch += 1
        self.seen = {e: {} for e in ENGS}
        for e, op in zip(ENGS, bops):
            self.ep_bounds[e].append(len(self.ops[e]))
            op.tok = ("e", e, len(self.ops[e]))
            self.ops[e].append(op)
        if False:
            op = Op("sp", None, kind="clear")
            op.arg = (self.epoch + 1) % 3
            op.tok = ("e", "sp", len(self.ops["sp"]))
            self.ops["sp"].append(op)

    def emit(self):
        import bisect
        nc = self.nc
        self.barrier()
        nep = self.epoch + 1
        need = {e: set() for e in ENGS}
        for e in ENGS:
            for op in self.ops[e]:
                for (k, v, ep) in op.waits:
                    if k[0] == "e":
                        need[k[1]].add(v)
            for op in self.ops[e]:
                if op.kind in ("barrier", "clear"):
                    need[e].add(op.tok[2])
        ep_of = lambda e, idx: bisect.bisect_right(self.ep_bounds[e], idx) - 1
        val = {e: {} for e in ENGS}
        used = [0] * nep
        per_ep = defaultdict(lambda: defaultdict(list))
        for e in ENGS:
            for idx in sorted(need[e]):
                per_ep[ep_of(e, idx)][e].append(idx)
        maxuse = 0
        base = 0
        for e in ENGS:
            lst = sorted(need[e])
            for r, idx in enumerate(lst):
                val[e][idx] = (0, base + r // SEM_MAX, r % SEM_MAX + 1)
            base += (len(lst) + SEM_MAX - 1) // SEM_MAX
        assert base <= self.n_esem, f"need {base} engine semaphores > {self.n_esem}"
        maxuse = base
        self.stats = {e: (len(self.ops[e]), len(need[e])) for e in ENGS}
        self.stats["epochs"] = nep
        self.stats["max_esem"] = maxuse
        with ExitStack() as es:
            esem = [[es.enter_context(nc.semaphore(f"e{s}_{j}")) for j in range(self.n_esem)] for s in range(1)]
            dsem = [[es.enter_context(nc.semaphore(f"d{s}_{j}")) for j in range(self.n_dsem)] for s in range(1)]
            block = es.enter_context(nc.Block())

            def run(E):
                def body(eng):
                    for idx, op in enumerate(self.ops[E]):
                        for (k, v, ep) in op.waits:
                            if k[0] == "e":
                                st, si, sv = val[k[1]][v]
                                eng.wait_ge(esem[st][si], sv)
                            else:
                                eng.wait_ge(dsem[0][k[1]], v)
                        if op.kind == "ins":
                            ins = op.fn(eng)
                        elif op.kind == "barrier":
                            ins = eng.nop()
                        else:
                            if DO_CLEAR:
                                for j in range(self.n_esem):
                                    eng.sem_clear(esem[op.arg][j])
                                for j in range(self.n_dsem):
                                    eng.sem_clear(dsem[op.arg][j])
                            ins = eng.nop()
                        if op.dsem is not None:
                            ins.then_inc(dsem[0][op.dsem[1]], 16)
                        elif idx in val[E]:
                            st, si, sv = val[E][idx]
                            ins.then_inc(esem[st][si], 1)
                return body

            block.tensor(run("pe"))
            block.scalar(run("act"))
            block.vector(run("dve"))
            block.gpsimd(run("pool"))
            block.sync(run("sp"))


class Arena:
    def __init__(self, big, size):
        self.big = big
        self.size = size
        self.top = 0
        self.stack = []
        self.peak = 0

    def push(self):
        self.stack.append(self.top)

    def pop(self):
        self.top = self.stack.pop()

    def alloc(self, free_shape, dtype, parts=128):
        n = 1
        for s in free_shape:
            n *= s
        nb = n * ESZ[dtype]
        nb_al = (nb + 63) // 64 * 64
        off = self.top
        assert off + nb_al <= self.size, f"SBUF arena overflow: need {off + nb_al} > {self.size}"
        self.top += nb_al
        self.peak = max(self.peak, self.top)
        v = self.big[0:parts, off:off + nb].bitcast(dtype)
        if len(free_shape) == 1:
            return v
        names = " ".join(f"d{i}" for i in range(len(free_shape)))
        kw = {f"d{i}": free_shape[i] for i in range(1, len(free_shape))}
        return v.rearrange(f"p ({names}) -> p {names}", **kw)


import numpy as np
import ml_dtypes
from contextlib import ExitStack
import concourse.bass as bass
import concourse.mybir as mybir
from concourse.bass_utils import run_bass_kernel_spmd

D = 1024
S = 2048
CT = 256
NT = 18
NTOK = NT * 128
EPS = 1e-6
INC = 1968
DFF = 2816
DFE = 3584
NEG = -30000.0
ARENA = 207 * 1024
SAME_SYNC = True
N_DUMMY_C = 0
XQ = "sp"

NG, CQG, CKVG, QG, KG, LNG, LNB, GB, RB = 0, 256, 512, 640, 1408, 2176, 2432, 2688, 2704
NBC = 2712
NCOLS = 68


def v3(ap, a):
    return ap.rearrange("p (a b) -> p a b", a=a)


def build(nlayers=2, stop=None, dbg=False, with_moe=True):
    nc = bass.Bass("TRN2", target_bir_lowering=False)

    def din(name, shape, dt=F32):
        return nc.dram_tensor(name, shape, dt, kind="ExternalInput").ap()

    def dscr(name, shape, dt=F32):
        return nc.dram_tensor(name, shape, dt).ap()

    xin = din("xin", [2, S, D])
    cin = din("cin", [2, CT, D])
    cT_d = din("cT", [128, 24])
    consts_d = din("consts", [128, 770])
    sel_d = din("sel", [4, 1024])
    rope_d = din("rope", [128, 16 * 32])
    ada_w = din("ada_w", [2, D, 6 * D])
    w_in = din("w_in", [2, D, INC])
    w_out = din("w_out", [2, D, D])
    w_uq = din("w_uq", [2, 256, 768])
    w_ukv = din("w_ukv", [2, 128, 1024])
    gws = din("gws", [2, 128, 512])
    bc_d = din("bc", [2, 128, NBC])
    cols_d = din("cols", [2, 128, NCOLS])
    rows_d = din("rows", [2, 1, 6 * D])
    ffn_w1 = din("ffn_w1", [D, DFF])
    ffn_w3 = din("ffn_w3", [D, DFF])
    ffn_w2 = din("ffn_w2", [DFF, D])
    if with_moe:
        moe_w1 = din("moe_w1", [8, D, DFE])
        moe_w3 = din("moe_w3", [8, D, DFE])
        moe_w2 = din("moe_w2", [8, DFE, D])
        rw_d = din("rw", [128, 64])
    out_d = nc.dram_tensor("out", [2, S, D], F32, kind="ExternalOutput").ap()
    dbg_d = {}
    if dbg:
        for nm, shp in (("d_mixA", [128, 2 * NTOK]), ("d_mixB", [64, 8 * NTOK]), ("d_mixC", [128, 2 * NTOK]),
                        ("d_mod", [128, 144]), ("d_g1b", [128, 3 * 1024])):
            dbg_d[nm] = nc.dram_tensor(nm, shp, F32, kind="ExternalOutput").ap()
        dbg_d["d_xmid"] = nc.dram_tensor("d_xmid", [NTOK, D], F32, kind="ExternalOutput").ap()
        dbg_d["d_x1"] = nc.dram_tensor("d_x1", [2, S, D], F32, kind="ExternalOutput").ap()
        dbg_d["d_xc1"] = nc.dram_tensor("d_xc1", [2, CT, D], F32, kind="ExternalOutput").ap()

    x1_d = dbg_d["d_x1"] if dbg else dscr("x1", [2, S, D])
    xc1_d = dbg_d["d_xc1"] if dbg else dscr("xc1", [2, CT, D])
    xmid_d = dscr("xmid", [NT, 128, D])
    ml_qk_d = dscr("ml_qk", [NT, 128, 512], BF16)
    ml_kv_d = dscr("ml_kv", [NT, 128, 516], BF16)
    ml_og_d = dscr("ml_og", [NT, 128, 272])
    qt_d = dscr("qt", [96, 8, NTOK], BF16)
    kt_d = dscr("kt", [96, 8, NTOK], BF16)
    v1_d = dscr("v1", [NT, 128, 520], BF16)

    es = ExitStack()
    big = es.enter_context(nc.sbuf_tensor("big", [128, ARENA], U8))
    ps = es.enter_context(nc.psum_tensor("ps", [128, 4096], F32))
    psb = ps[:, :].bitcast(BF16)
    A = Arena(big, ARENA)
    P = Prog(nc, same_eng_sync=SAME_SYNC)

    def PSF(bank, a, b, parts=128, p0=0):
        return ps[p0:p0 + parts, bank * 512 + a: bank * 512 + b]

    def PSB(bank, a, b, parts=128):
        return psb[0:parts, bank * 1024 + a: bank * 1024 + b]

    dve = lambda m, **kw: P.I("dve", m, **kw)
    act = lambda m, **kw: P.I("act", m, **kw)
    pe = lambda m, **kw: P.I("pe", m, **kw)
    pool = lambda m, **kw: P.I("pool", m, **kw)

    def mm(out, lhsT, rhs, start=True, stop=True):
        pe("matmul", out=out, lhsT=lhsT, rhs=rhs, start=start, stop=stop)

    def rstd_from_ss(out, ss, n, tmp):
        act("activation", out=tmp, in_=ss, func=AF.Ln, scale=1.0 / n, bias=EPS)
        act("activation", out=out, in_=tmp, func=AF.Exp, scale=-0.5)

    consts = A.alloc([770], F32)
    P.dma("sp", consts, consts_d)
    identf = consts[:, 0:128]
    tri = [consts[:, 128:256], consts[:, 256:384]]
    mask = [consts[:, 384:512], consts[:, 512:640]]
    ones_f = consts[:, 640:768]
    hmask = consts[:, 768:770]
    identb = A.alloc([128], BF16)
    dve("tensor_copy", out=identb, in_=identf)
    selt = A.alloc([1024], F32, parts=4)
    P.dma("sp", selt, sel_d)
    sel4 = v3(selt[:, 0:512], 4)
    sel4n = v3(selt[:, 512:1024], 4)
    ropet = A.alloc([16, 32], F32)
    P.dma("sp", ropet, v3(rope_d, 16))
    sm = A.alloc([64], F32)
    junkb = A.alloc([1024], BF16)
    junkf = A.alloc([256], F32)

    for l in range(nlayers):
        last = l == 1
        A.push()
        bcL = A.alloc([NBC], F32)
        P.dma("sp", bcL, bc_d[l])
        colsL = A.alloc([NCOLS], F32)
        P.dma("sp", colsL, cols_d[l])
        modT = A.alloc([48, 3], F32)
        scl = A.alloc([2, 8, 3], F32)
        g1b = A.alloc([3, 1024], F32)
        g2b = A.alloc([3, 1024], F32)

        A.push()
        sT = A.alloc([24], F32)
        P.dma("sp", sT, cT_d)
        act("activation", out=sT, in_=sT, func=AF.Silu)
        brow = A.alloc([6 * D], F32, parts=1)
        P.dma("sp", brow, rows_d[l])
        modrow = A.alloc([6 * D], F32, parts=3)
        NWB = 4
        wbl = [A.alloc([8, 512], F32) for _ in range(NWB)]
        for jb in range(12):
            wb = wbl[jb % NWB]
            P.dma("sp" if jb % 2 == 0 else "act", wb,
                  ada_w[l][:, jb * 512:(jb + 1) * 512].rearrange("(kc p) n -> p kc n", p=128))
            bank = 1 + jb % 3
            o_ = PSF(bank, 0, 512, parts=3)
            for kc in range(8):
                mm(o_, sT[:, kc * 3:kc * 3 + 3], wb[:, kc, :], start=kc == 0, stop=False)
            mm(o_, ones_f[0:1, 0:3], brow[0:1, jb * 512:(jb + 1) * 512], start=False, stop=True)
            act("copy", out=modrow[:, jb * 512:(jb + 1) * 512], in_=o_)
            for jj in range(4):
                j = jb * 4 + jj
                pe("transpose", out=PSF(0, j * 3, j * 3 + 3), in_=modrow[0:3, j * 128:(j + 1) * 128], identity=identf[0:3, 0:3])
            if jb in (4, 5, 10, 11):
                gt = g1b if jb < 6 else g2b
                half = jb % 2
                for r in range(3):
                    bk = 4 + r
                    mm(PSF(bk, 0, 512), sel4[0:3, r, :], modrow[0:3, jb * 512:(jb + 1) * 512])
                    act("copy", out=gt[:, r, half * 512:(half + 1) * 512], in_=PSF(bk, 0, 512))
        dve("tensor_copy", out=modT, in_=v3(PSF(0, 0, 144), 48))
        for k, (jsc, gcol) in enumerate(((8, 0), (32, 8))):
            dve("tensor_scalar_add", out=scl[:, k], in0=modT[:, jsc:jsc + 8, :], scalar1=1.0)
            dve("tensor_tensor", out=scl[:, k], in0=scl[:, k],
                in1=colsL[:, gcol:gcol + 8].unsqueeze(2).to_broadcast([128, 8, 3]), op=ALU.mult)
        if dbg and l == 0:
            P.dma("sp", dbg_d["d_mod"], modT.rearrange("p a b -> p (a b)"))
            P.dma("sp", dbg_d["d_g1b"], g1b.rearrange("p a b -> p (a b)"))
        A.pop()
        P.barrier()
        if stop == "ada":
            break

        for b in range(2):
            A.push()
            hmT = A.alloc([8, NTOK], BF16)
            gate_all = A.alloc([16, 8], F32)
            A.push()
            mixAT = A.alloc([2, NTOK], BF16)
            mixCT = A.alloc([2, NTOK], BF16)
            tiles = list(range(NT))

            def src_tile(t):
                if t < 2:
                    base = cin if l == 0 else xc1_d
                    return base[b, t * 128:(t + 1) * 128, :]
                base = xin if l == 0 else x1_d
                return base[b, (t - 2) * 128:(t - 1) * 128, :]

            def norm_mod_T(xt, k, r, outT, tcols, f32path=False, hTf=None):
                ss = sm[:, 0:1]
                act("activation", out=junkb, in_=xt, func=AF.Square, accum_out=ss)
                rstd_from_ss(sm[:, 1:2], ss, D, sm[:, 2:3])
                shift_j = 0 if k == 0 else 24
                if not f32path:
                    xn = A_xn
                    dve("tensor_scalar_mul", out=xn, in0=xt, scalar1=sm[:, 1:2])
                    for kc in range(8):
                        pe("transpose", out=PSB(0, kc * 128, (kc + 1) * 128), in_=xn[:, kc * 128:(kc + 1) * 128], identity=identb)
                    for kc in range(8):
                        act("activation", out=outT[:, kc, tcols[0]:tcols[1]], in_=PSB(0, kc * 128, (kc + 1) * 128),
                            func=AF.Identity, scale=scl[:, k, kc, r:r + 1], bias=modT[:, shift_j + kc, r:r + 1])
                else:
                    xn = A_xnf
                    dve("tensor_scalar_mul", out=xn, in0=xt, scalar1=sm[:, 1:2])
                    for kc in range(8):
                        pe("transpose", out=PSF(2 + kc // 4, (kc % 4) * 128, (kc % 4 + 1) * 128),
                           in_=xn[:, kc * 128:(kc + 1) * 128], identity=identf)
                    for kc in range(8):
                        act("activation", out=hTf[:, kc, :], in_=PSF(2 + kc // 4, (kc % 4) * 128, (kc % 4 + 1) * 128),
                            func=AF.Identity, scale=scl[:, k, kc, r:r + 1], bias=modT[:, shift_j + kc, r:r + 1])
                    dve("tensor_copy", out=outT[:, :, tcols[0]:tcols[1]], in_=hTf)

            A.push()
            w_in_sb = A.alloc([8, INC], BF16)
            for kc in range(8):
                P.dma("pool", w_in_sb[:, kc, :], w_in[l][kc * 128:(kc + 1) * 128, :])
            w_uq_sb = A.alloc([2, 768], BF16)
            P.dma("pool", w_uq_sb, w_uq[l].rearrange("(c p) n -> p c n", p=128))
            w_ukv_sb = A.alloc([1024], BF16)
            P.dma("pool", w_ukv_sb, w_ukv[l])
            gws_sb = A.alloc([4, 128], BF16)
            P.dma("pool", gws_sb, v3(gws[l], 4))
            xbuf = [A.alloc([D], F32) for _ in range(2)]
            A_xn = A.alloc([D], BF16)
            xmT = A.alloc([8, 128], BF16)
            qk_bf = A.alloc([512], BF16)
            qkT_st = [A.alloc([512], BF16) for _ in range(2)]
            kv_st = [A.alloc([516], BF16) for _ in range(2)]
            og_st = [A.alloc([272], F32) for _ in range(2)]
            v1_st = [A.alloc([8, 65], BF16) for _ in range(2)]
            for i in range(2):
                dve("memset", ap=kv_st[i], constant=1.0)
                dve("memset", ap=v1_st[i], constant=1.0)
            gt = A.alloc([16], F32)
            ef = A.alloc([8], F32)
            cqn = A.alloc([256], BF16)
            cqT = A.alloc([2, 128], BF16)
            ckvn = A.alloc([128], BF16)
            ckvT = A.alloc([128], BF16)
            krs = A.alloc([32], F32)
            ga = A.alloc([512], F32)
            bst = A.alloc([int(nc.vector.BN_STATS_DIM)], F32)
            bag = A.alloc([int(nc.vector.BN_AGGR_DIM)], F32)
            vn_f = A.alloc([256], F32)
            vn_bf = A.alloc([256], BF16)
            cf = A.alloc([256], F32)
            c_bf = A.alloc([256], BF16)
            qs = A.alloc([768], F32)
            sq = A.alloc([768], F32)
            qn = A.alloc([8, 96], F32)
            rtmp = A.alloc([4, 8, 16], F32)
            qr_bf = A.alloc([8, 96], BF16)
            qT_st = [A.alloc([8, 128], BF16, parts=96) for _ in range(2)]
            kvs = A.alloc([8, 128], F32)
            kn = A.alloc([8, 96], F32)
            kr_bf = A.alloc([8, 96], BF16)
            kT_st = [A.alloc([8, 128], BF16, parts=96) for _ in range(2)]
            s8 = A.alloc([32], F32)
            s8k = A.alloc([32], F32)
            sqk = A.alloc([512], F32)
            rtmpk = A.alloc([4, 8, 16], F32)

            def rope_apply(src, dst, tl):
                cosb = ropet[:, tl, 0:16].unsqueeze(1).to_broadcast([128, 8, 16])
                sinb = ropet[:, tl, 16:32].unsqueeze(1).to_broadcast([128, 8, 16])
                t1 = src[:, :, 64:80]
                t2 = src[:, :, 80:96]
                dve("tensor_tensor", out=rtmp[:, 0], in0=t1, in1=cosb, op=ALU.mult)
                dve("tensor_tensor", out=rtmp[:, 1], in0=t2, in1=sinb, op=ALU.mult)
                dve("tensor_tensor", out=rtmp[:, 2], in0=t1, in1=sinb, op=ALU.mult)
                dve("tensor_tensor", out=rtmp[:, 3], in0=t2, in1=cosb, op=ALU.mult)
                dve("tensor_tensor", out=dst[:, :, 64:80], in0=rtmp[:, 0], in1=rtmp[:, 1], op=ALU.subtract)
                dve("tensor_tensor", out=dst[:, :, 80:96], in0=rtmp[:, 2], in1=rtmp[:, 3], op=ALU.add)
                dve("tensor_copy", out=dst[:, :, 0:64], in_=src[:, :, 0:64])

            zc0 = [0, 512, 1024, 1456]
            zc1 = [512, 1024, 1456, 1968]

            def front(t):
                r = 2 if t < 2 else b
                xt = xbuf[t % 2]
                P.dma(XQ, xt, src_tile(t))
                norm_mod_T(xt, 0, r, xmT, (0, 128))
                for kc in range(8):
                    for g in range(4):
                        mm(PSF(1 + g, 0, zc1[g] - zc0[g]), xmT[:, kc, :], w_in_sb[:, kc, zc0[g]:zc1[g]],
                           start=kc == 0, stop=kc == 7)

            def evac(t):
                pp = t % 2
                need_c = not (last and t < 2)
                act("copy", out=qk_bf, in_=PSF(1, 0, 512))
                dve("tensor_copy", out=v3(kv_st[pp][:, 256:516], 4)[:, :, 0:64], in_=v3(PSF(2, 0, 256), 4))
                act("activation", out=og_st[pp][:, 0:256], in_=PSF(2, 256, 512), func=AF.Exp, scale=-1.0)
                dve("tensor_tensor", out=gt, in0=PSF(3, 0, 16), in1=bcL[:, GB:GB + 16], op=ALU.add)
                act("activation", out=junkf, in_=PSF(3, 16, 272), func=AF.Square, accum_out=sm[:, 4:5])
                act("activation", out=junkf[:, 0:128], in_=PSF(3, 272, 400), func=AF.Square, accum_out=sm[:, 8:9])
                act("copy", out=krs, in_=PSF(3, 400, 432))
                rstd_from_ss(sm[:, 5:6], sm[:, 4:5], 256, sm[:, 6:7])
                rstd_from_ss(sm[:, 9:10], sm[:, 8:9], 128, sm[:, 10:11])
                dve("scalar_tensor_tensor", out=cqn, in0=PSF(3, 16, 272), scalar=sm[:, 5:6], in1=bcL[:, CQG:CQG + 256],
                    op0=ALU.mult, op1=ALU.mult)
                dve("scalar_tensor_tensor", out=ckvn, in0=PSF(3, 272, 400), scalar=sm[:, 9:10], in1=bcL[:, CKVG:CKVG + 128],
                    op0=ALU.mult, op1=ALU.mult)
                if need_c:
                    act("activation", out=ga, in_=PSF(4, 0, 512), func=AF.Gelu_apprx_tanh)

            def back(t):
                pp = t % 2
                need_q = not (last and t < 2)
                need_c = need_q

                def chain_m():
                    for i in range(4):
                        pe("transpose", out=PSB(5, i * 128, (i + 1) * 128), in_=qk_bf[:, i * 128:(i + 1) * 128], identity=identb)
                    yield
                    dve("tensor_copy", out=qkT_st[pp], in_=PSB(5, 0, 512))
                    P.dma("sp", ml_qk_d[t], qkT_st[pp], WK=[("mlqk", t)])
                    pool("tensor_copy", out=kv_st[pp][:, 0:256], in_=qk_bf[:, 256:512])
                    yield
                    dve("tensor_scalar_add", out=og_st[pp][:, 0:256], in0=og_st[pp][:, 0:256], scalar1=1.0)
                    act("activation", out=ef, in_=gt[:, 8:16], func=AF.Exp, scale=-1.0)
                    yield
                    dve("reciprocal", out=og_st[pp][:, 0:256], in_=og_st[pp][:, 0:256])
                    act("activation", out=ef, in_=ef, func=AF.Ln, bias=1.0)
                    P.dma("sp", ml_kv_d[t], kv_st[pp], WK=[("mlkv", t)])
                    pool("tensor_copy", out=og_st[pp][:, 256:264], in_=gt[:, 0:8])
                    yield
                    dve("tensor_scalar_mul", out=og_st[pp][:, 264:272], in0=ef, scalar1=-1.0)
                    P.dma("sp", ml_og_d[t], og_st[pp], WK=[("mlog", t)])
                    yield

                def chain_pre():
                    for c in range(2):
                        pe("transpose", out=PSB(5, 512 + c * 128, 640 + c * 128), in_=cqn[:, c * 128:(c + 1) * 128], identity=identb)
                    pe("transpose", out=PSB(5, 768, 896), in_=ckvn, identity=identb)
                    yield
                    dve("tensor_copy", out=cqT, in_=v3(PSB(5, 512, 768), 2))
                    dve("tensor_copy", out=ckvT, in_=PSB(5, 768, 896))
                    yield
                    if need_q:
                        for c in range(2):
                            mm(PSF(6, 0, 512), cqT[:, c, :], w_uq_sb[:, c, 0:512], start=c == 0, stop=c == 1)
                        for c in range(2):
                            mm(PSF(7, 0, 256), cqT[:, c, :], w_uq_sb[:, c, 512:768], start=c == 0, stop=c == 1)
                        yield
                        act("copy", out=qs[:, 0:512], in_=PSF(6, 0, 512))
                        act("copy", out=qs[:, 512:768], in_=PSF(7, 0, 256))
                        yield
                    mm(PSF(6, 0, 512), ckvT, w_ukv_sb[:, 0:512])
                    mm(PSF(7, 0, 512), ckvT, w_ukv_sb[:, 512:1024])
                    yield
                    act("copy", out=kvs[:, 0:4, :], in_=v3(PSF(6, 0, 512), 4))
                    act("copy", out=kvs[:, 4:8, :], in_=v3(PSF(7, 0, 512), 4))
                    yield

                def chain_g():
                    if not need_c:
                        return
                    dve("bn_stats", out=bst, in_=ga[:, 256:512])
                    dve("bn_aggr", out=bag, in_=bst)
                    yield
                    rstd_from_ss(sm[:, 13:14], bag[:, 1:2], 1.0, sm[:, 12:13])
                    yield
                    dve("tensor_scalar", out=vn_f, in0=ga[:, 256:512], scalar1=bag[:, 0:1], scalar2=sm[:, 13:14],
                        op0=ALU.subtract, op1=ALU.mult)
                    yield
                    dve("tensor_tensor", out=vn_f, in0=vn_f, in1=bcL[:, LNG:LNG + 256], op=ALU.mult)
                    yield
                    dve("tensor_tensor", out=vn_bf, in0=vn_f, in1=bcL[:, LNB:LNB + 256], op=ALU.add)
                    yield
                    for g in range(4):
                        mm(PSF(5, g * 64, (g + 1) * 64), gws_sb[:, g, :], vn_bf[:, g * 64:(g + 1) * 64])
                    yield
                    dve("tensor_tensor", out=v3(cf, 4), in0=v3(PSF(5, 0, 256), 4),
                        in1=colsL[:, 64:68].unsqueeze(2).to_broadcast([128, 4, 64]), op=ALU.add)
                    yield
                    dve("tensor_tensor", out=c_bf, in0=cf, in1=ga[:, 0:256], op=ALU.mult)
                    yield
                    for c in range(2):
                        pe("transpose", out=PSB(5, 512 + c * 128, 640 + c * 128), in_=c_bf[:, c * 128:(c + 1) * 128], identity=identb)
                    yield
                    dve("tensor_copy", out=mixCT[:, :, t * 128:(t + 1) * 128], in_=v3(PSB(5, 512, 768), 2))
                    yield

                def rope_gen(src, dst, tl, tmp):
                    cosb = ropet[:, tl, 0:16].unsqueeze(1).to_broadcast([128, 8, 16])
                    sinb = ropet[:, tl, 16:32].unsqueeze(1).to_broadcast([128, 8, 16])
                    t1 = src[:, :, 64:80]
                    t2 = src[:, :, 80:96]
                    dve("tensor_tensor", out=tmp[:, 0], in0=t1, in1=cosb, op=ALU.mult)
                    yield
                    dve("tensor_tensor", out=tmp[:, 1], in0=t2, in1=sinb, op=ALU.mult)
                    yield
                    dve("tensor_tensor", out=tmp[:, 2], in0=t1, in1=sinb, op=ALU.mult)
                    yield
                    dve("tensor_tensor", out=tmp[:, 3], in0=t2, in1=cosb, op=ALU.mult)
                    yield
                    dve("tensor_tensor", out=dst[:, :, 64:80], in0=tmp[:, 0], in1=tmp[:, 1], op=ALU.subtract)
                    yield
                    dve("tensor_tensor", out=dst[:, :, 80:96], in0=tmp[:, 2], in1=tmp[:, 3], op=ALU.add)
                    yield
                    dve("tensor_copy", out=dst[:, :, 0:64], in_=src[:, :, 0:64])
                    yield

                def chain_q():
                    if not need_q:
                        return
                    dve("tensor_tensor", out=sq, in0=qs, in1=qs, op=ALU.mult)
                    yield
                    dve("tensor_reduce", out=s8[:, 0:8], in_=v3(sq, 8), axis=AX.X, op=ALU.add)
                    yield
                    rstd_from_ss(s8[:, 8:16], s8[:, 0:8], 96, s8[:, 16:24])
                    yield
                    dve("tensor_tensor", out=qn, in0=v3(qs, 8), in1=s8[:, 8:16].unsqueeze(2).to_broadcast([128, 8, 96]), op=ALU.mult)
                    yield
                    if t >= 2:
                        dve("tensor_tensor", out=qn, in0=qn, in1=v3(bcL[:, QG:QG + 768], 8), op=ALU.mult)
                        yield
                        yield from rope_gen(qn, qr_bf, t - 2, rtmp)
                    else:
                        dve("tensor_tensor", out=qr_bf, in0=qn, in1=v3(bcL[:, QG:QG + 768], 8), op=ALU.mult)
                        yield
                    for h in range(8):
                        pe("transpose", out=PSB(6, h * 128, (h + 1) * 128, parts=96), in_=qr_bf[:, h, :], identity=identb)
                    yield
                    act("copy", out=qT_st[pp], in_=v3(PSB(6, 0, 1024, parts=96), 8))
                    P.dma("sp", qt_d[:, :, t * 128:(t + 1) * 128], qT_st[pp], WK=[("qt", t)])
                    yield

                def chain_k():
                    pool("tensor_copy", out=v1_st[pp][:, :, 0:64], in_=kvs[:, :, 64:128])
                    P.dma("sp", v1_d[t], v1_st[pp].rearrange("p a b -> p (a b)"), WK=[("v1", t)])
                    dve("tensor_tensor", out=v3(sqk, 8), in0=kvs[:, :, 0:64], in1=kvs[:, :, 0:64], op=ALU.mult)
                    act("activation", out=junkf[:, 0:32], in_=krs, func=AF.Square, accum_out=sm[:, 16:17])
                    yield
                    dve("tensor_reduce", out=s8k[:, 0:8], in_=v3(sqk, 8), axis=AX.X, op=ALU.add)
                    yield
                    dve("tensor_scalar_add", out=s8k[:, 0:8], in0=s8k[:, 0:8], scalar1=sm[:, 16:17])
                    yield
                    rstd_from_ss(s8k[:, 8:16], s8k[:, 0:8], 96, s8k[:, 16:24])
                    yield
                    rkb = s8k[:, 8:16]
                    dve("tensor_tensor", out=kn[:, :, 0:64], in0=kvs[:, :, 0:64], in1=rkb.unsqueeze(2).to_broadcast([128, 8, 64]), op=ALU.mult)
                    yield
                    dve("tensor_tensor", out=kn[:, :, 64:96], in0=krs.unsqueeze(1).to_broadcast([128, 8, 32]),
                        in1=rkb.unsqueeze(2).to_broadcast([128, 8, 32]), op=ALU.mult)
                    yield
                    if t >= 2:
                        dve("tensor_tensor", out=kn, in0=kn, in1=v3(bcL[:, KG:KG + 768], 8), op=ALU.mult)
                        yield
                        yield from rope_gen(kn, kr_bf, t - 2, rtmpk)
                    else:
                        dve("tensor_tensor", out=kr_bf, in0=kn, in1=v3(bcL[:, KG:KG + 768], 8), op=ALU.mult)
                        yield
                    for h in range(8):
                        pe("transpose", out=PSB(7, h * 128, (h + 1) * 128, parts=96), in_=kr_bf[:, h, :], identity=identb)
                    yield
                    act("copy", out=kT_st[pp], in_=v3(PSB(7, 0, 1024, parts=96), 8))
                    P.dma("sp", kt_d[:, :, t * 128:(t + 1) * 128], kT_st[pp], WK=[("kt", t)])
                    yield

                def rr(gens):
                    gens = list(gens)
                    while gens:
                        for g_ in list(gens):
                            try:
                                next(g_)
                            except StopIteration:
                                gens.remove(g_)

                gm, gg = chain_m(), chain_g()
                live = [gm, gg]
                gp = chain_pre()
                while True:
                    try:
                        next(gp)
                    except StopIteration:
                        break
                    for g_ in list(live):
                        try:
                            next(g_)
                        except StopIteration:
                            live.remove(g_)
                rr(live + [chain_q(), chain_k()])

            front(tiles[0])
            for idx, t in enumerate(tiles):
                evac(t)
                if idx + 1 < len(tiles):
                    front(tiles[idx + 1])
                back(t)
            A.pop()
            P.barrier()
            if dbg and l == 0 and b == 0:
                P.dma("sp", dbg_d["d_mixC"].bitcast(BF16)[:, 0:2 * NTOK], mixCT.rearrange("p a b -> p (a b)"))
            if stop == "A":
                break

            A.push()
            Cn_f = A.alloc([2, 2, 65], F32)
            Cn_bf = A.alloc([2, 2, 65], BF16)
            mst = A.alloc([8], F32)
            dve("memset", ap=Cn_f, constant=0.0)
            dve("memset", ap=Cn_bf, constant=0.0)
            dve("memset", ap=mst, constant=0.0)
            hsum = A.alloc([NT, 256], F32)
            NB_ = 2

            class VS:
                pass
            vs = []
            for d in range(2):
                v = VS()
                v.qkb = [A.alloc([512], BF16) for _ in range(NB_)]
                v.kvb = [A.alloc([516], BF16) for _ in range(NB_)]
                v.ogb = [A.alloc([272], F32) for _ in range(NB_)]
                v.bb = A.alloc([8], F32)
                v.u = A.alloc([4], F32)
                v.uT = A.alloc([128], F32, parts=4)
                v.M1T = A.alloc([128], F32, parts=4)
                v.tmpU = A.alloc([4, 128], F32)
                v.cm = A.alloc([4], F32)
                v.cml = A.alloc([4], F32)
                v.mx = A.alloc([4], F32)
                v.M1 = A.alloc([4], F32)
                v.E4 = A.alloc([16], F32)
                v.X4 = A.alloc([16], F32)
                v.ET = A.alloc([512], BF16)
                v.PT = A.alloc([512], BF16)
                v.t1 = A.alloc([4, 65], F32)
                v.tot = A.alloc([4, 65], F32)
                v.dd = A.alloc([4], F32)
                v.rec = A.alloc([4], F32)
                v.t2 = A.alloc([4, 64], F32)
                v.kw = A.alloc([4, 64], BF16)
                v.sqh = A.alloc([4, 64], F32)
                v.an = A.alloc([4, 64], F32)
                v.a_bf = A.alloc([256], BF16)
                v.fs = A.alloc([16], F32)
                v.qm = A.alloc([4, 128], BF16)
                v.B = 4 * d
                vs.append(v)
            fwd = list(range(NT))
            bwd = [1, 0] + list(range(NT - 1, 1, -1))
            visited = set()

            def stage1(v, d, t, step, with_h):
                bi = step % NB_
                v.qk, v.kv, v.og = v.qkb[bi], v.kvb[bi], v.ogb[bi]
                P.dma("sp", v.qk, ml_qk_d[t], RK=[("mlqk", t)])
                yield
                P.dma("sp", v.kv, ml_kv_d[t], RK=[("mlkv", t)])
                yield
                P.dma("sp", v.og, ml_og_d[t], RK=[("mlog", t)])
                yield
                B0 = v.B
                f_ = v.og[:, 264 + 4 * d:268 + 4 * d]
                i_ = v.og[:, 256 + 4 * d:260 + 4 * d]
                mm(PSF(B0, 0, 4), tri[d], f_)
                yield
                mm(PSF(B0, 4, 8), ones_f, f_)
                yield
                dve("tensor_copy", out=v.bb, in_=PSF(B0, 0, 8))
                yield
                dve("tensor_tensor", out=v.u, in0=i_, in1=v.bb[:, 0:4], op=ALU.subtract)
                yield
                pe("transpose", out=PSF(B0, 16, 144, parts=4), in_=v.u, identity=identf)
                yield
                act("copy", out=v.uT, in_=PSF(B0, 16, 144, parts=4))
                yield
                for h in range(4):
                    mm(PSF(B0 + 1, h * 128, (h + 1) * 128), sel4[:, h, :], v.uT)
                    yield
                if with_h:
                    for half in range(2):
                        dve("tensor_scalar_mul", out=v.qm[:, half:4:2, :], in0=v3(v.qk[:, 0:256], 2), scalar1=hmask[:, half:half + 1])
                    for h in range(4):
                        mm(PSF(B0 + 3, h * 65, (h + 1) * 65), v.qm[:, h, :], Cn_bf[:, d, h // 2, :])

            def stage2(v, d, t, step, with_h):
                B0 = v.B
                md = mst[:, 4 * d:4 * d + 4]
                if with_h:
                    dve("tensor_tensor", out=v.tmpU, in0=v3(PSF(B0 + 1, 0, 512), 4),
                        in1=mask[1 - d].unsqueeze(1).to_broadcast([128, 4, 128]), op=ALU.add)
                    yield
                    dve("tensor_reduce", out=v.cm, in_=v.tmpU, axis=AX.X, op=ALU.max)
                    yield
                dve("tensor_reduce", out=v.cml, in_=v3(PSF(B0 + 1, 0, 512), 4), axis=AX.X, op=ALU.max)
                yield
                dve("tensor_tensor", out=v.mx, in0=md, in1=v.cml, op=ALU.max)
                yield
                dve("tensor_tensor", out=v.E4[:, 8:12], in0=md, in1=v.mx, op=ALU.subtract)
                yield
                dve("tensor_tensor", out=v.E4[:, 12:16], in0=v.u, in1=v.mx, op=ALU.subtract)
                yield
                if with_h:
                    dve("tensor_tensor", out=v.M1, in0=md, in1=v.cm, op=ALU.max)
                    yield
                    dve("tensor_tensor", out=v.E4[:, 0:4], in0=md, in1=v.M1, op=ALU.subtract)
                    yield
                    dve("scalar_tensor_tensor", out=v.E4[:, 4:8], in0=v.bb[:, 0:4], scalar=-1.0, in1=v.M1,
                        op0=ALU.mult, op1=ALU.subtract)
                    yield
                    act("activation", out=v.X4, in_=v.E4, func=AF.Exp)
                    yield
                    pe("transpose", out=PSF(B0, 144, 272, parts=4), in_=v.M1, identity=identf)
                    yield
                    act("copy", out=v.M1T, in_=PSF(B0, 144, 272, parts=4))
                    yield
                    for h in range(4):
                        o_ = PSF(B0 + 2, h * 128, (h + 1) * 128)
                        mm(o_, v.uT, sel4[:, h, :], start=True, stop=False)
                        mm(o_, sel4n[:, h, :], v.M1T, start=False, stop=False)
                        mm(o_, identf, mask[d], start=False, stop=True)
                    for h in range(4):
                        mm(PSF(B0 + 1, h * 128, (h + 1) * 128), v.qk[:, (2 + h // 2) * 128:(3 + h // 2) * 128], v.qm[:, h, :])
                else:
                    act("activation", out=v.X4[:, 8:16], in_=v.E4[:, 8:16], func=AF.Exp)
                    yield
                dve("tensor_tensor", out=md, in0=v.bb[:, 4:8], in1=v.mx, op=ALU.add)
                yield

            def stage3(v, d, t, step, with_h):
                B0 = v.B
                if not with_h:
                    return
                a_, emt = v.X4[:, 0:4], v.X4[:, 4:8]
                v1 = v3(v.kv[:, 256:516], 4)
                act("activation", out=v.ET, in_=PSF(B0 + 2, 0, 512), func=AF.Exp)
                yield
                dve("scalar_tensor_tensor", out=v.PT, in0=PSF(B0 + 1, 0, 512), scalar=0.125, in1=v.ET, op0=ALU.mult, op1=ALU.mult)
                yield
                for h in range(4):
                    mm(PSF(B0 + 2, h * 65, (h + 1) * 65), v.PT[:, h * 128:(h + 1) * 128], v1[:, h, :])
                    yield
                dve("tensor_tensor", out=v.t1, in0=v3(PSF(B0 + 3, 0, 260), 4), in1=a_.unsqueeze(2).to_broadcast([128, 4, 65]), op=ALU.mult)
                yield
                dve("tensor_tensor", out=v.tot, in0=v3(PSF(B0 + 2, 0, 260), 4), in1=v.t1, op=ALU.add)
                yield
                den = v.tot[:, :, 64]
                dve("scalar_tensor_tensor", out=v.dd, in0=den, scalar=-1.0, in1=den, op0=ALU.mult, op1=ALU.max)
                yield
                dve("tensor_tensor", out=v.dd, in0=v.dd, in1=emt, op=ALU.max)
                yield
                dve("reciprocal", out=v.rec, in_=v.dd)
                yield
                hs = v3(hsum[:, t, :], 4)
                recb = v.rec.unsqueeze(2).to_broadcast([128, 4, 64])
                if t not in visited:
                    dve("tensor_tensor", out=hs, in0=v.tot[:, :, 0:64], in1=recb, op=ALU.mult)
                    yield
                else:
                    dve("tensor_tensor", out=v.t2, in0=v.tot[:, :, 0:64], in1=recb, op=ALU.mult)
                    yield
                    dve("tensor_tensor", out=hs, in0=hs, in1=v.t2, op=ALU.add)
                    yield
                    dve("tensor_tensor", out=v.sqh, in0=hs, in1=hs, op=ALU.mult)
                    yield
                    dve("tensor_reduce", out=v.fs[:, 0:4], in_=v.sqh, axis=AX.X, op=ALU.add)
                    yield
                    rstd_from_ss(v.fs[:, 4:8], v.fs[:, 0:4], 64, v.fs[:, 8:12])
                    yield
                    dve("tensor_tensor", out=v.an, in0=hs, in1=v.fs[:, 4:8].unsqueeze(2).to_broadcast([128, 4, 64]), op=ALU.mult)
                    yield
                    dve("tensor_tensor", out=v.an, in0=v.an, in1=v3(bcL[:, NG:NG + 256], 4), op=ALU.mult)
                    yield
                    dve("tensor_tensor", out=v3(v.a_bf, 4), in0=v.an, in1=v3(v.og[:, 0:256], 4), op=ALU.mult)
                    yield
                    for c in range(2):
                        pe("transpose", out=PSB(B0, 768 + c * 128, 896 + c * 128), in_=v.a_bf[:, c * 128:(c + 1) * 128], identity=identb)
                    dve("tensor_copy", out=mixAT[:, :, t * 128:(t + 1) * 128], in_=v3(PSB(B0, 768, 1024), 2))
                    yield
                visited.add(t)

            def stage4(v, d, t, step, with_h):
                B0 = v.B
                w_ = v.X4[:, 12:16]
                k3 = v3(v.kv[:, 0:256], 4)
                v1 = v3(v.kv[:, 256:516], 4)
                dve("scalar_tensor_tensor", out=v.kw, in0=k3, scalar=0.125, in1=w_.unsqueeze(2).to_broadcast([128, 4, 64]),
                    op0=ALU.mult, op1=ALU.mult)
                yield
                kwf = v.kw.rearrange("p a b -> p (a b)")
                for h in range(4):
                    mm(PSF(B0 + 1, h * 65, (h + 1) * 65), kwf[:, (h // 2) * 128:(h // 2 + 1) * 128], v1[:, h, :])
                    yield
                for half in range(2):
                    p0 = half * 64
                    psv = v3(PSF(B0 + 1, 0, 260, parts=64, p0=p0), 4)[:, half:4:2, :]
                    cst = Cn_f[p0:p0 + 64, d]
                    dve("tensor_tensor", out=cst, in0=cst,
                        in1=v.X4[p0:p0 + 64, 8 + half:12:2].unsqueeze(2).to_broadcast([64, 2, 65]), op=ALU.mult)
                    yield
                    dve("tensor_tensor", out=cst, in0=cst, in1=psv, op=ALU.add)
                    yield
                    act("copy", out=Cn_bf[p0:p0 + 64, d], in_=cst)
                    yield

            def visit_gen(v, d, t, step, with_h):
                for stg in (stage1, stage2, stage3, stage4):
                    g_ = stg(v, d, t, step, with_h)
                    if g_ is not None:
                        yield from g_

            for step in range(NT):
                gens = []
                for d in range(2):
                    t = (fwd if d == 0 else bwd)[step]
                    gens.append(visit_gen(vs[d], d, t, step, not (last and t < 2)))
                while gens:
                    for g_ in list(gens):
                        try:
                            next(g_)
                        except StopIteration:
                            gens.remove(g_)
            A.pop()
            if dbg and l == 0 and b == 0:
                P.dma("sp", dbg_d["d_mixA"].bitcast(BF16)[:, 0:2 * NTOK], mixAT.rearrange("p a b -> p (a b)"))
            if stop == "B":
                break

            mixBT = A.alloc([8, NTOK], BF16, parts=64)
            A.push()
            QTb = [A.alloc([NTOK], BF16, parts=96) for _ in range(2)]
            KTb = [A.alloc([NTOK], BF16, parts=96) for _ in range(2)]
            V1 = A.alloc([NT, 520], BF16)
            P.dma("sp", V1, v1_d.rearrange("t p f -> p t f"), RK=[("v1", t_) for t_ in range(NT)])
            PTb = [A.alloc([512], BF16) for _ in range(4)]
            SBK = [0, 1, 2, 7]
            Os = [A.alloc([512], F32, parts=65) for _ in range(2)]
            recr = A.alloc([512], F32)
            scl_att = 96.0 ** -0.5
            blocks = []
            if not last:
                blocks.append((0, 256, [0, 1]))
            for qb in range(4):
                blocks.append((256 + qb * 512, 512, list(range(NT))))
            scnt = 0
            ocnt = 0
            pending = [None]

            def make_tail(h, q0, nq, bankO, bankB, osb):
                def tail():
                    dve("tensor_copy", out=osb[:, 0:nq], in_=PSF(bankO, 0, nq, parts=65))
                    dve("reciprocal", out=recr[64:65, 0:nq], in_=osb[64:65, 0:nq])
                    mm(PSF(bankB, 0, nq, parts=64), ones_f[64:65, 0:64], recr[64:65, 0:nq])
                    dve("tensor_tensor", out=mixBT[:, h, q0:q0 + nq], in0=osb[0:64, 0:nq], in1=PSF(bankB, 0, nq, parts=64), op=ALU.mult)
                return tail

            items = []
            oc = 0
            for h in range(8):
                for (q0, nq, kts) in blocks:
                    for i, kt in enumerate(kts):
                        items.append((h, q0, nq, kt, i, len(kts), oc))
                    oc += 1
            loaded = set()

            def load_head(h):
                if h < 8 and h not in loaded:
                    loaded.add(h)
                    P.dma("sp", QTb[h % 2], qt_d[:, h, :], RK=[("qt", t_) for t_ in range(NT)])
                    P.dma("sp", KTb[h % 2], kt_d[:, h, :], RK=[("kt", t_) for t_ in range(NT)])

            def S_item(j):
                h, q0, nq, kt, i, nk, oc_ = items[j]
                load_head(h)
                mm(PSF(SBK[j % 4], 0, nq), KTb[h % 2][:, kt * 128:(kt + 1) * 128], QTb[h % 2][:, q0:q0 + nq])

            NI = len(items)
            for j in range(min(3, NI)):
                S_item(j)
            for j, (h, q0, nq, kt, i, nk, oc_) in enumerate(items):
                bankO = 3 + oc_ % 2
                bankB = 5 + oc_ % 2
                osb = Os[oc_ % 2]
                pb = PTb[j % 4]
                act("activation", out=pb[:, 0:nq], in_=PSF(SBK[j % 4], 0, nq), func=AF.Exp, scale=scl_att)
                mm(PSF(bankO, 0, nq, parts=65), V1[:, kt, h * 65:(h + 1) * 65], pb[:, 0:nq], start=i == 0, stop=i == nk - 1)
                if j + 3 < NI:
                    S_item(j + 3)
                if i == min(1, nk - 1) and pending[0] is not None:
                    pending[0]()
                    pending[0] = None
                if i == nk - 1:
                    if pending[0] is not None:
                        pending[0]()
                    pending[0] = make_tail(h, q0, nq, bankO, bankB, osb)
            if pending[0] is not None:
                pending[0]()
            A.pop()
            if dbg and l == 0 and b == 0:
                P.dma("sp", dbg_d["d_mixB"].bitcast(BF16)[:, 0:8 * NTOK], mixBT.rearrange("p a b -> p (a b)"))
            if stop == "C":
                break

            A.push()
            woA = A.alloc([2, D], BF16)
            woB = A.alloc([8, D], BF16, parts=64)
            woC = A.alloc([2, D], BF16)
            P.dma("pool", woA, w_out[l][0:256, :].rearrange("(c p) n -> p c n", p=128))
            P.dma("pool", woB, w_out[l][256:768, :].rearrange("(h p) n -> p h n", p=64))
            P.dma("pool", woC, w_out[l][768:1024, :].rearrange("(c p) n -> p c n", p=128))
            xbuf = [A.alloc([D], F32) for _ in range(2)]
            xmb = [A.alloc([D], F32) for _ in range(2)]
            A_xn = A.alloc([D], BF16)
            if last:
                A_xnf = A.alloc([D], F32)
                hTf = A.alloc([8, 128], F32)
                rw_sb = A.alloc([8, 8], F32)
                P.dma("sp", rw_sb, v3(rw_d, 8))
                lg = A.alloc([8], F32)
                l2 = A.alloc([8], F32)
                eq1 = A.alloc([8], F32)
                eq2 = A.alloc([8], F32)
                tp = A.alloc([16], F32)
            d1_tiles = [t for t in tiles if not (last and t < 2)]

            def d1_mm(t):
                pp = t % 2
                P.dma("sp", xbuf[pp], src_tile(t))
                tc0, tc1 = t * 128, (t + 1) * 128
                for cg in range(2):
                    o_ = PSF(4 + 2 * pp + cg, 0, 512)
                    cs = slice(cg * 512, (cg + 1) * 512)
                    chunks = [(mixAT[:, 0, tc0:tc1], woA[:, 0, cs]), (mixAT[:, 1, tc0:tc1], woA[:, 1, cs])]
                    chunks += [(mixBT[:, h, tc0:tc1], woB[:, h, cs]) for h in range(8)]
                    chunks += [(mixCT[:, 0, tc0:tc1], woC[:, 0, cs]), (mixCT[:, 1, tc0:tc1], woC[:, 1, cs])]
                    for i, (lt, rh) in enumerate(chunks):
                        mm(o_, lt, rh, start=i == 0, stop=i == len(chunks) - 1)

            def d1_evac(t):
                r = 2 if t < 2 else b
                pp = t % 2
                xt, xm = xbuf[pp], xmb[pp]
                dve("tensor_tensor", out=xm, in0=ps[:, (4 + 2 * pp) * 512:(6 + 2 * pp) * 512], in1=g1b[:, r, :], op=ALU.mult)
                dve("tensor_tensor", out=xm, in0=xm, in1=xt, op=ALU.add)
                P.dma("sp", xmid_d[t], xm, WK=[("xmid", t)])
                if dbg and l == 0 and b == 0:
                    P.dma("sp", dbg_d["d_xmid"][t * 128:(t + 1) * 128, :], xm)

            def d1_norm(t):
                r = 2 if t < 2 else b
                xm = xmb[t % 2]
                tc0, tc1 = t * 128, (t + 1) * 128
                if not last:
                    norm_mod_T(xm, 1, r, hmT, (tc0, tc1))
                else:
                    norm_mod_T(xm, 1, r, hmT, (tc0, tc1), f32path=True, hTf=hTf)
                    for kc in range(8):
                        mm(PSF(1, 0, 8), hTf[:, kc, :], rw_sb[:, kc, :], start=kc == 0, stop=kc == 7)
                    dve("tensor_tensor", out=lg, in0=PSF(1, 0, 8), in1=bcL[:, RB:RB + 8], op=ALU.add)
                    dve("tensor_reduce", out=tp[:, 0:1], in_=lg, axis=AX.X, op=ALU.max)
                    dve("tensor_tensor", out=eq1, in0=lg, in1=tp[:, 0:1].to_broadcast([128, 8]), op=ALU.is_equal)
                    dve("scalar_tensor_tensor", out=l2, in0=eq1, scalar=-1e30, in1=lg, op0=ALU.mult, op1=ALU.add)
                    dve("tensor_reduce", out=tp[:, 1:2], in_=l2, axis=AX.X, op=ALU.max)
                    dve("tensor_tensor", out=eq2, in0=l2, in1=tp[:, 1:2].to_broadcast([128, 8]), op=ALU.is_equal)
                    dve("tensor_tensor", out=tp[:, 2:3], in0=tp[:, 1:2], in1=tp[:, 0:1], op=ALU.subtract)
                    act("activation", out=tp[:, 3:4], in_=tp[:, 2:3], func=AF.Exp)
                    dve("tensor_scalar_add", out=tp[:, 4:5], in0=tp[:, 3:4], scalar1=1.0)
                    dve("reciprocal", out=tp[:, 5:6], in_=tp[:, 4:5])
                    dve("tensor_tensor", out=tp[:, 6:7], in0=tp[:, 3:4], in1=tp[:, 5:6], op=ALU.mult)
                    gsl = gate_all[:, t - 2, :]
                    dve("tensor_scalar_mul", out=gsl, in0=eq1, scalar1=tp[:, 5:6])
                    dve("scalar_tensor_tensor", out=gsl, in0=eq2, scalar=tp[:, 6:7], in1=gsl, op0=ALU.mult, op1=ALU.add)

            d1_mm(d1_tiles[0])
            for i_, t in enumerate(d1_tiles):
                d1_evac(t)
                if i_ + 1 < len(d1_tiles):
                    d1_mm(d1_tiles[i_ + 1])
                d1_norm(t)
            A.pop()
            A.pop()
            P.barrier()
            if stop == "D1":
                break

            A.push()
            y_acc = A.alloc([NT, D], F32)
            G = 2
            if not last:
                experts = [None]
                nff = DFF // 128
                tok_blocks = [(0, 512), (512, 512), (1024, 512), (1536, 512), (2048, 256)]
            else:
                experts = list(range(8)) if with_moe else []
                nff = DFE // 128
                tok_blocks = [(256 + i * 512, 512) for i in range(4)]
            A.push()
            w1b = [A.alloc([8, G * 128], BF16) for _ in range(2)]
            w3b = [A.alloc([8, G * 128], BF16) for _ in range(2)]
            w2b = [A.alloc([G, D], BF16) for _ in range(2)]
            gTb = [A.alloc([G, NTOK], BF16) for _ in range(2)]
            s1b = [A.alloc([512], F32) for _ in range(2)]
            groups = []
            for e in experts:
                for gi in range(nff // G):
                    groups.append((e, gi))
            cnt = {"b": 0, "y": 0}

            def up_gen(gidx):
                e, gi = groups[gidx]
                if e is None:
                    W1, W3, W2 = ffn_w1, ffn_w3, ffn_w2
                else:
                    W1, W3, W2 = moe_w1[e], moe_w3[e], moe_w2[e]
                w1g, w3g, w2g, gT = w1b[gidx % 2], w3b[gidx % 2], w2b[gidx % 2], gTb[gidx % 2]
                c0, c1 = gi * G * 128, (gi + 1) * G * 128
                P.dma("pool", w1g, W1[:, c0:c1].rearrange("(kc p) n -> p kc n", p=128))
                P.dma("pool", w3g, W3[:, c0:c1].rearrange("(kc p) n -> p kc n", p=128))
                P.dma("pool", w2g, W2[c0:c1, :].rearrange("(f p) n -> p f n", p=128))
                for f in range(G):
                    for (q0, n) in tok_blocks:
                        bk = cnt["b"] % 2
                        cnt["b"] += 1
                        for kc in range(8):
                            mm(PSF(bk, 0, n), w1g[:, kc, f * 128:(f + 1) * 128], hmT[:, kc, q0:q0 + n], start=kc == 0, stop=kc == 7)
                        for kc in range(8):
                            mm(PSF(2 + bk, 0, n), w3g[:, kc, f * 128:(f + 1) * 128], hmT[:, kc, q0:q0 + n], start=kc == 0, stop=kc == 7)
                        s1 = s1b[bk]
                        act("activation", out=s1[:, 0:n], in_=PSF(bk, 0, n), func=AF.Silu)
                        dve("tensor_tensor", out=gT[:, f, q0:q0 + n], in0=PSF(2 + bk, 0, n), in1=s1[:, 0:n], op=ALU.mult)
                        yield

            def down_gen(gidx):
                e, gi = groups[gidx]
                w2g, gT = w2b[gidx % 2], gTb[gidx % 2]
                first = (gidx == 0)
                for ti, t in enumerate(d1_tiles):
                    by = 4 + 2 * (cnt["y"] % 2)
                    cnt["y"] += 1
                    for cg in range(2):
                        for f in range(G):
                            mm(PSF(by + cg, 0, 512), gT[:, f, t * 128:(t + 1) * 128], w2g[:, f, cg * 512:(cg + 1) * 512],
                               start=f == 0, stop=f == G - 1)
                    ya = y_acc[:, t, :]
                    psy = ps[:, by * 512:(by + 2) * 512]
                    if e is None:
                        if first:
                            act("copy", out=ya, in_=psy)
                        else:
                            dve("tensor_tensor", out=ya, in0=psy, in1=ya, op=ALU.add)
                    else:
                        gsc = gate_all[:, t - 2, e:e + 1]
                        if first:
                            dve("tensor_scalar_mul", out=ya, in0=psy, scalar1=gsc)
                        else:
                            dve("scalar_tensor_tensor", out=ya, in0=psy, scalar=gsc, in1=ya,
                                op0=ALU.mult, op1=ALU.add)
                    if ti % 2 == 1:
                        yield

            def run_rr(gens):
                gens = [g_ for g_ in gens if g_ is not None]
                while gens:
                    for g_ in list(gens):
                        try:
                            next(g_)
                        except StopIteration:
                            gens.remove(g_)

            if groups:
                run_rr([up_gen(0)])
                for gidx in range(len(groups)):
                    nxt = up_gen(gidx + 1) if gidx + 1 < len(groups) else None
                    run_rr([nxt, down_gen(gidx)])
            A.pop()
            xbuf = [A.alloc([D], F32) for _ in range(2)]
            for t in d1_tiles:
                r = 2 if t < 2 else b
                xt = xbuf[t % 2]
                P.dma("sp", xt, xmid_d[t], RK=[("xmid", t)])
                if experts:
                    dve("tensor_tensor", out=y_acc[:, t, :], in0=y_acc[:, t, :], in1=g2b[:, r, :], op=ALU.mult)
                    dve("tensor_tensor", out=xt, in0=xt, in1=y_acc[:, t, :], op=ALU.add)
                if last:
                    dst = out_d[b, (t - 2) * 128:(t - 1) * 128, :]
                elif t < 2:
                    dst = xc1_d[b, t * 128:(t + 1) * 128, :]
                else:
                    dst = x1_d[b, (t - 2) * 128:(t - 1) * 128, :]
                P.dma("act", dst, xt)
            A.pop()
            P.barrier()
            A.pop()
        else:
            A.pop()
            continue
        break
    P.emit()
    print("ops", P.stats, "sbuf peak", A.peak)
    return nc, es


def host_consts():
    idn = np.eye(128, dtype=np.float32)
    s = np.arange(128)[:, None]
    t = np.arange(128)[None, :]
    triF = (s <= t).astype(np.float32)
    triB = (s >= t).astype(np.float32)
    maskF = np.where(s <= t, 0.0, NEG).astype(np.float32)
    maskB = np.where(s >= t, 0.0, NEG).astype(np.float32)
    ones = np.ones((128, 128), np.float32)
    hm = np.zeros((128, 2), np.float32)
    hm[:64, 0] = 1.0
    hm[64:, 1] = 1.0
    consts = np.concatenate([idn, triF, triB, maskF, maskB, ones, hm], axis=1)
    sel = np.zeros((4, 2, 4, 128), np.float32)
    for h in range(4):
        sel[h, 0, h, :] = 1.0
        sel[h, 1, h, :] = -1.0
    sel = sel.reshape(4, 1024)
    rows = S // 64
    row = np.repeat(np.arange(rows), 64).astype(np.float32)
    col = np.tile(np.arange(64), rows).astype(np.float32)
    inv = (np.float32(10000.0) ** (-np.arange(8, dtype=np.float32) / np.float32(8))).astype(np.float32)
    ang = np.concatenate([row[:, None] * inv, col[:, None] * inv], axis=-1).astype(np.float32)
    cs = np.concatenate([np.cos(ang), np.sin(ang)], axis=-1).astype(np.float32)
    rope = cs.reshape(16, 128, 32).transpose(1, 0, 2).reshape(128, 512)
    return consts, sel, np.ascontiguousarray(rope)


def make_in_maps(inp, ncores=8, with_moe=True):
    consts, sel, rope = host_consts()
    f = lambda a: np.ascontiguousarray(np.asarray(a, dtype=np.float32))
    L = 2
    bc = np.zeros((L, 128, NBC), np.float32)
    cols = np.zeros((L, 128, NCOLS), np.float32)
    for l in range(L):
        row = np.concatenate([
            inp["mlstm_norm_g"][l], inp["mla_cq_g"][l], inp["mla_ckv_g"][l],
            np.tile(inp["mla_q_g"][l], 8), np.tile(inp["mla_k_g"][l], 8),
            inp["gmlp_ln_g"][l], inp["gmlp_ln_b"][l], inp["mlstm_gate_b"][l],
            inp["moe_router_b"][0] if l == 1 else np.zeros(8, np.float32)]).astype(np.float32)
        assert row.shape[0] == NBC
        bc[l] = np.broadcast_to(row[None, :], (128, NBC))
        cols[l, :, 0:8] = inp["norm1_g"][l].reshape(8, 128).T
        cols[l, :, 8:16] = inp["norm2_g"][l].reshape(8, 128).T
        cols[l, :, 16:64] = inp["ada_b"][l].reshape(48, 128).T
        cols[l, :, 64:68] = inp["gmlp_b_s"][l].T
    gws = np.ascontiguousarray(np.transpose(inp["gmlp_w_s"], (0, 3, 1, 2))).reshape(L, 128, 512)
    rows = f(inp["ada_b"]).reshape(L, 1, 6 * D)
    rw = f(inp["moe_router_w"][0]).reshape(8, 128, 8).transpose(1, 0, 2).reshape(128, 64)
    shared = {
        "consts": consts, "sel": sel, "rope": rope,
        "ada_w": f(inp["ada_w"]), "w_in": f(inp["w_in"]), "w_out": f(inp["w_out"]),
        "w_uq": f(inp["mla_w_uq"]), "w_ukv": f(inp["mla_w_ukv"]), "gws": f(gws),
        "bc": bc, "cols": cols, "rows": rows,
        "ffn_w1": f(inp["ffn_w1"][0]), "ffn_w3": f(inp["ffn_w3"][0]), "ffn_w2": f(inp["ffn_w2"][0]),
    }
    if with_moe:
        shared.update({"moe_w1": f(inp["moe_w1"][0]), "moe_w3": f(inp["moe_w3"][0]), "moe_w2": f(inp["moe_w2"][0]),
                       "rw": np.ascontiguousarray(rw)})
    maps = []
    for c in range(ncores):
        cc = np.stack([inp["c"][2 * c], inp["c"][2 * c + 1], inp["c_ctx"]]).astype(np.float32)
        cT = cc.reshape(3, 8, 128).transpose(2, 1, 0).reshape(128, 24)
        m = dict(shared)
        m["xin"] = f(inp["x"][2 * c:2 * c + 2])
        m["cin"] = f(inp["ctx"][2 * c:2 * c + 2])
        m["cT"] = np.ascontiguousarray(cT)
        maps.append(m)
    return maps


def kernel(**inp):
    nc, es = build()
    maps = make_in_maps(inp)
    res = run_bass_kernel_spmd(nc, maps, core_ids=list(range(8)))
    es.close()
    return np.concatenate([r["out"] for r in res.results], axis=0)
```
